# Optimizing a Trainium2 kernel written in Bass

```python
import jax, jax.numpy as jnp
from jax import lax
import numpy as np

D_MODEL = 2048
BATCH = 4
SEQ = 8192
DEPTH = 2

HEAD_DIM = 128
ATTN_GROUPS = ((128, 1), (512, 4), (2048, 16))
N_GROUPS = 3
HEADS_PER_GROUP = 4
N_ATTN_HEADS = N_GROUPS * HEADS_PER_GROUP
ATTN_WIDTH = N_ATTN_HEADS * HEAD_DIM
ROPE_DIM = HEAD_DIM // 4
ROPE_THETA = 500000.0
CONV_CH = D_MODEL
CONV_WIDTH = 31
D_FF = 7168
N_EXPERTS = 8
TOP_K = 2
BLOCK = 128
EPS = 1e-6
NEG_INF = -1e30
IN_COLS = 3 * ATTN_WIDTH + 2 * CONV_CH + 2 * D_MODEL
N_DENSE = (DEPTH + 1) // 2
N_MOE = DEPTH // 2

kernel_name = "hybrid_dilated_attn_conformer_moe"


def rmsnorm(t, g):
    tf = t.astype(jnp.float32)
    y = tf * lax.rsqrt(jnp.mean(tf * tf, axis=-1, keepdims=True) + EPS)
    return (y * g.astype(jnp.float32)).astype(t.dtype)


def layernorm(t, g, b):
    tf = t.astype(jnp.float32)
    mu = jnp.mean(tf, axis=-1, keepdims=True)
    var = jnp.mean(jnp.square(tf - mu), axis=-1, keepdims=True)
    y = (tf - mu) * lax.rsqrt(var + EPS)
    return (y * g.astype(jnp.float32) + b.astype(jnp.float32)).astype(t.dtype)


def partial_rope(t, pos):
    half = ROPE_DIM // 2
    inv = jnp.power(jnp.float32(ROPE_THETA), -jnp.arange(half, dtype=jnp.float32) * 2.0 / ROPE_DIM)
    ang = pos[:, None] * inv[None, :]
    cos = jnp.cos(ang)[None, :, None, :]
    sin = jnp.sin(ang)[None, :, None, :]
    tf = t.astype(jnp.float32)
    x1 = tf[..., :half]
    x2 = tf[..., half:ROPE_DIM]
    out = jnp.concatenate([x1 * cos - x2 * sin, x2 * cos + x1 * sin, tf[..., ROPE_DIM:]], axis=-1)
    return out.astype(t.dtype)


def dilated_window_attention(q, k, v, window, dilation):
    b, s, h, e = q.shape
    span = window // dilation
    L = s // dilation
    pad = (-L) % BLOCK
    Lp = L + pad
    nb = Lp // BLOCK

    def strided(t):
        t = t.reshape(b, L, dilation, h, e).transpose(0, 2, 3, 1, 4)
        t = jnp.pad(t, ((0, 0), (0, 0), (0, 0), (0, pad), (0, 0)))
        return t.reshape(b, dilation, h, nb, BLOCK, e)

    def with_prev(t):
        prev = jnp.pad(t[:, :, :, :-1], ((0, 0), (0, 0), (0, 0), (1, 0), (0, 0), (0, 0)))
        return jnp.concatenate([prev, t], axis=4)

    qb = strided(q)
    kk = with_prev(strided(k))
    vv = with_prev(strided(v))
    scores = jnp.einsum('bdhnqe,bdhnke->bdhnqk', qb, kk,
                        preferred_element_type=jnp.float32) * (e ** -0.5)
    n_i = jnp.arange(nb)[:, None, None]
    q_i = jnp.arange(BLOCK)[None, :, None]
    k_i = jnp.arange(2 * BLOCK)[None, None, :]
    dist = BLOCK + q_i - k_i
    key_idx = (n_i - 1) * BLOCK + k_i
    mask = (dist >= 0) & (dist <= span) & (key_idx >= 0)
    scores = jnp.where(mask, scores, NEG_INF)
    lse = jax.nn.logsumexp(scores, axis=-1)
    p = jnp.exp(scores - lse[..., None]).astype(v.dtype)
    o = jnp.einsum('bdhnqk,bdhnke->bdhnqe', p, vv)
    o = o.reshape(b, dilation, h, Lp, e)[:, :, :, :L].transpose(0, 3, 1, 2, 4).reshape(b, s, h, e)
    lse = lse.reshape(b, dilation, h, Lp)[..., :L].transpose(0, 3, 1, 2).reshape(b, s, h)
    return o, lse


def swiglu(h, w1, w3, w2):
    return (jax.nn.silu(h @ w1) * (h @ w3)) @ w2


def moe_swiglu(h, router, w1, w3, w2):
    logits = jnp.einsum('bsd,de->bse', h, router, preferred_element_type=jnp.float32)
    top_v, top_i = lax.top_k(logits, TOP_K)
    top_w = jax.nn.softmax(top_v, axis=-1)
    gates = jnp.sum(jax.nn.one_hot(top_i, N_EXPERTS, dtype=jnp.float32) * top_w[..., None], axis=-2)
    gates = gates.astype(h.dtype)
    y = jnp.zeros_like(h)
    for e in range(N_EXPERTS):
        y = y + gates[..., e:e + 1] * swiglu(h, w1[e], w3[e], w2[e])
    return y


def hybrid_mixer(h, w_in, q_g, k_g, w_attn_o, conv_w, conv_b, ln_g, ln_b, w_conv_o, gate_b, w_out):
    b, s, _ = h.shape
    proj = h @ w_in
    o0 = 0
    q = proj[..., o0:o0 + ATTN_WIDTH].reshape(b, s, N_ATTN_HEADS, HEAD_DIM); o0 += ATTN_WIDTH
    k = proj[..., o0:o0 + ATTN_WIDTH].reshape(b, s, N_ATTN_HEADS, HEAD_DIM); o0 += ATTN_WIDTH
    v = proj[..., o0:o0 + ATTN_WIDTH].reshape(b, s, N_ATTN_HEADS, HEAD_DIM); o0 += ATTN_WIDTH
    c_val = proj[..., o0:o0 + CONV_CH]; o0 += CONV_CH
    c_gate = proj[..., o0:o0 + CONV_CH]; o0 += CONV_CH
    g_attn = proj[..., o0:o0 + D_MODEL]; o0 += D_MODEL
    g_conv = proj[..., o0:o0 + D_MODEL]

    pos = jnp.arange(s, dtype=jnp.float32)
    q = partial_rope(rmsnorm(q, q_g), pos)
    k = partial_rope(rmsnorm(k, k_g), pos)
    outs, lses = [], []
    for g, (window, dilation) in enumerate(ATTN_GROUPS):
        hs = slice(g * HEADS_PER_GROUP, (g + 1) * HEADS_PER_GROUP)
        o_g, lse_g = dilated_window_attention(q[:, :, hs], k[:, :, hs], v[:, :, hs], window, dilation)
        outs.append(o_g)
        lses.append(lse_g)
    alpha = jax.nn.softmax(jnp.stack(lses, axis=0), axis=0).astype(v.dtype)
    o = jnp.concatenate([outs[g] * alpha[g][..., None] for g in range(N_GROUPS)], axis=2)
    y_attn = o.reshape(b, s, ATTN_WIDTH) @ w_attn_o

    u = c_val * jax.nn.sigmoid(c_gate)
    u = lax.conv_general_dilated(u, conv_w.astype(u.dtype), window_strides=(1,),
                                 padding=((CONV_WIDTH - 1, 0),),
                                 dimension_numbers=('NWC', 'WIO', 'NWC'),
                                 feature_group_count=CONV_CH) + conv_b
    u = jax.nn.silu(layernorm(u, ln_g, ln_b))
    y_conv = u @ w_conv_o

    merged = jax.nn.sigmoid(g_attn + gate_b[:D_MODEL]) * y_attn + jax.nn.sigmoid(g_conv + gate_b[D_MODEL:]) * y_conv
    return merged @ w_out


def setup_inputs(seed: int = 0) -> dict:
    key = jax.random.key(seed)
    ks = jax.random.split(key, 24)
    f32 = jnp.float32
    nrm = lambda k, shape, scale: jax.random.normal(k, shape, f32) * scale
    return {
        "x": nrm(ks[0], (BATCH, SEQ, D_MODEL), 1.0),
        "norm_mix": 1.0 + nrm(ks[1], (DEPTH, D_MODEL), 0.02),
        "w_in": nrm(ks[2], (DEPTH, D_MODEL, IN_COLS), D_MODEL ** -0.5),
        "q_norm": 1.0 + nrm(ks[3], (DEPTH, HEAD_DIM), 0.02),
        "k_norm": 1.0 + nrm(ks[4], (DEPTH, HEAD_DIM), 0.02),
        "w_attn_o": nrm(ks[5], (DEPTH, ATTN_WIDTH, D_MODEL), ATTN_WIDTH ** -0.5),
        "conv_w": nrm(ks[6], (DEPTH, CONV_WIDTH, 1, CONV_CH), CONV_WIDTH ** -0.5),
        "conv_b": nrm(ks[7], (DEPTH, CONV_CH), 0.01),
        "conv_ln_g": 1.0 + nrm(ks[8], (DEPTH, CONV_CH), 0.02),
        "conv_ln_b": nrm(ks[9], (DEPTH, CONV_CH), 0.01),
        "w_conv_o": nrm(ks[10], (DEPTH, CONV_CH, D_MODEL), CONV_CH ** -0.5),
        "gate_b": nrm(ks[11], (DEPTH, 2 * D_MODEL), 0.01),
        "w_out": nrm(ks[12], (DEPTH, D_MODEL, D_MODEL), D_MODEL ** -0.5),
        "norm_ffn": 1.0 + nrm(ks[13], (DEPTH, D_MODEL), 0.02),
        "ffn_w1": nrm(ks[14], (N_DENSE, D_MODEL, D_FF), D_MODEL ** -0.5),
        "ffn_w3": nrm(ks[15], (N_DENSE, D_MODEL, D_FF), D_MODEL ** -0.5),
        "ffn_w2": nrm(ks[16], (N_DENSE, D_FF, D_MODEL), D_FF ** -0.5),
        "router": nrm(ks[17], (N_MOE, D_MODEL, N_EXPERTS), D_MODEL ** -0.5),
        "moe_w1": nrm(ks[18], (N_MOE, N_EXPERTS, D_MODEL, D_FF), D_MODEL ** -0.5),
        "moe_w3": nrm(ks[19], (N_MOE, N_EXPERTS, D_MODEL, D_FF), D_MODEL ** -0.5),
        "moe_w2": nrm(ks[20], (N_MOE, N_EXPERTS, D_FF, D_MODEL), D_FF ** -0.5),
    }


def reference(x, norm_mix, w_in, q_norm, k_norm, w_attn_o, conv_w, conv_b, conv_ln_g, conv_ln_b,
              w_conv_o, gate_b, w_out, norm_ffn, ffn_w1, ffn_w3, ffn_w2, router, moe_w1, moe_w3, moe_w2):
    for l in range(DEPTH):
        h = rmsnorm(x, norm_mix[l])
        x = x + hybrid_mixer(h, w_in[l], q_norm[l], k_norm[l], w_attn_o[l], conv_w[l], conv_b[l],
                             conv_ln_g[l], conv_ln_b[l], w_conv_o[l], gate_b[l], w_out[l])
        h = rmsnorm(x, norm_ffn[l])
        if l % 2 == 0:
            i = l // 2
            x = x + swiglu(h, ffn_w1[i], ffn_w3[i], ffn_w2[i])
        else:
            i = l // 2
            x = x + moe_swiglu(h, router[i], moe_w1[i], moe_w3[i], moe_w2[i])
    return x
```

```python
import numpy as np
from contextlib import ExitStack
import concourse.bass as bass
import concourse.mybir as mybir
from concourse.bass_utils import run_bass_kernel_spmd

F32 = mybir.dt.float32
BF16 = mybir.dt.bfloat16
AF = mybir.ActivationFunctionType
ALU = mybir.AluOpType

D = 2048
NC_ = 16
SEQ = 8192
BATCH = 4
HD = 128
NH = 12
AW = 1536
DFF = 7168
NE = 8
INC = 12800
EXT = 8192
OWN0 = 4096
HALO0 = 2048
EPS = 1e-6
DIL = (1, 4, 16)
CW = 31

PV_NMIX, PV_NFFN, PV_CB, PV_LG, PV_LB, PV_GB, PV_QN, PV_KN, PV_CWT = 0, 16, 32, 48, 64, 80, 112, 113, 114
PV_L = 114 + 16 * CW
CSTW = 640 + 128 + 256
MOE_C = 192


class Buf:
    __slots__ = ("w", "r", "semkey", "keep")

    def __init__(self, keep=False):
        self.w = {}
        self.r = {}
        self.semkey = None
        self.keep = keep


class Prog:
    ENG = ("pe", "act", "dve", "pool", "sp")

    def __init__(self, nc, es):
        self.nc = nc
        self.es = es
        self.q = {e: [] for e in self.ENG}
        self.tr = {e: [] for e in self.ENG}
        self.sem = {}
        self.known = {e: {} for e in self.ENG}
        self.pend = {e: [] for e in self.ENG}
        for e in ("pe", "act", "dve", "pool"):
            self.newsem(e)
        self.ndsem = 0
        self.ninst = 0
        self.free_dsem = []
        self.phase_dsem = []
        self.sb_top = 16512
        self.sb_mark = 16512
        self.nalloc = 0

    def newsem(self, key):
        h = self.es.enter_context(self.nc.semaphore("s_" + str(key)))
        self.sem[key] = [h, 0]

    def sb(self, name, shape, dt):
        nbytes = int(np.prod(shape[1:])) * (4 if dt == F32 else 2)
        off = (self.sb_top + 63) // 64 * 64
        assert off + nbytes <= 229376, ("SBUF overflow", name, off, nbytes)
        self.sb_top = off + nbytes
        self.nalloc += 1
        return self.nc.alloc_sbuf_tensor_at("%s_%d" % (name, self.nalloc), list(shape), dt, offset=off)

    def persist(self):
        self.sb_mark = self.sb_top

    def phase_end(self):
        self.barrier()
        self.sb_top = self.sb_mark
        self.free_dsem.extend(self.phase_dsem)
        self.phase_dsem = []

    def ps(self, name, shape, dt=F32):
        return self.es.enter_context(self.nc.psum_tensor(name, shape, dt))

    def _waits(self, eng, reads, writes, skip_own):
        need = {}
        for b in reads:
            for k, v in b.w.items():
                if need.get(k, 0) < v:
                    need[k] = v
        for b in writes:
            for k, v in b.w.items():
                if need.get(k, 0) < v:
                    need[k] = v
            for k, v in b.r.items():
                if need.get(k, 0) < v:
                    need[k] = v
        kn = self.known[eng]
        for k, v in need.items():
            if skip_own and k == eng:
                continue
            if kn.get(k, 0) < v:
                kn[k] = v
                h = self.sem[k][0]
                self.q[eng].append(lambda e, h=h, v=v: e.wait_ge(h, v))
                self.tr[eng].append(("w", k, v))
                self.ninst += 1

    def op(self, eng, fn, reads=(), writes=(), signal=True):
        self._waits(eng, reads, writes, skip_own=(eng == "pe"))
        s = self.sem[eng]
        val = s[1] + 1
        if signal:
            s[1] = val
            h = s[0]
            self.q[eng].append(lambda e, fn=fn, h=h: fn(e).then_inc(h, 1))
            self.tr[eng].append(("i", eng, 1))
        else:
            self.q[eng].append(fn)
        self.ninst += 1
        for b in reads:
            if b.r.get(eng, 0) < val:
                b.r[eng] = val
        for b in writes:
            if b.w.get(eng, 0) < val:
                b.w[eng] = val

    def dma(self, eng, fn, sbuf, reads=(), writes=()):
        if sbuf.semkey is None:
            if self.free_dsem:
                sbuf.semkey = self.free_dsem.pop()
            else:
                sbuf.semkey = "d%d" % self.ndsem
                self.ndsem += 1
                self.newsem(sbuf.semkey)
            if not sbuf.keep:
                self.phase_dsem.append(sbuf.semkey)
        self._waits(eng, reads, writes, skip_own=False)
        s = self.sem[sbuf.semkey]
        s[1] += 16
        val = s[1]
        h = s[0]
        self.q[eng].append(lambda e, fn=fn, h=h: fn(e).then_inc(h, 16))
        self.tr[eng].append(("i", sbuf.semkey, 16))
        self.ninst += 1
        k = sbuf.semkey
        for b in reads:
            if b.r.get(k, 0) < val:
                b.r[k] = val
        for b in writes:
            if b.w.get(k, 0) < val:
                b.w[k] = val

    def barrier(self):
        for eng in self.ENG:
            kn = self.known[eng]
            for k, (h, v) in self.sem.items():
                if v > 0 and kn.get(k, 0) < v:
                    kn[k] = v
                    self.q[eng].append(lambda e, h=h, v=v: e.wait_ge(h, v))
                    self.tr[eng].append(("w", k, v))

    def simulate(self):
        cnt = {k: 0 for k in self.sem}
        ptr = {e: 0 for e in self.ENG}
        while True:
            prog = False
            for e in self.ENG:
                t = self.tr[e]
                p = ptr[e]
                while p < len(t):
                    kind, k, v = t[p]
                    if kind == "i":
                        cnt[k] += v
                    elif cnt[k] < v:
                        break
                    p += 1
                if p != ptr[e]:
                    prog = True
                    ptr[e] = p
            if not prog:
                break
        bad = {e: (ptr[e], len(self.tr[e]), self.tr[e][ptr[e]], cnt[self.tr[e][ptr[e]][1]])
               for e in self.ENG if ptr[e] < len(self.tr[e])}
        return bad

    def emit(self):
        with self.nc.Block() as block:
            q = self.q

            @block.tensor
            def _(e):
                for f in q["pe"]:
                    f(e)

            @block.scalar
            def _(e):
                for f in q["act"]:
                    f(e)

            @block.vector
            def _(e):
                for f in q["dve"]:
                    f(e)

            @block.gpsimd
            def _(e):
                for f in q["pool"]:
                    f(e)

            @block.sync
            def _(e):
                for f in q["sp"]:
                    f(e)


class Rot:
    def __init__(self, P, name, n, shape, dt, psum=False):
        self.t = [(P.ps if psum else P.sb)("%s%d" % (name, i), shape, dt) for i in range(n)]
        self.b = [Buf() for _ in range(n)]
        self.i = 0
        self.n = n

    def next(self):
        i = self.i
        self.i = (i + 1) % self.n
        return self.t[i], self.b[i]


def build_program(stop_after=None, debug=False, layers=(0, 1), use_ffn=True, use_moe=True, dbg_blocks=None, dbg_range=None, dbg_moe_l0=False, sparse=True):
    nc = bass.Bass("TRN2", target_bir_lowering=False)
    es = ExitStack()
    P = Prog(nc, es)
    kind_s = "ExternalOutput" if debug else "Internal"

    def din(name, shape, dt=F32):
        return nc.dram_tensor(name, list(shape), dt, kind="ExternalInput").ap()

    def dscr(name, shape, dt):
        return nc.dram_tensor(name, list(shape), dt, kind=kind_s).ap()

    xT_in = din("xT", [D, EXT])
    hmask_in = din("hmask", [128, 1])
    cos_in = din("cosT", [128, EXT])
    sin_in = din("sinT", [128, EXT])
    cst_in = din("cst", [128, CSTW])
    pvec_in = din("pvec", [128, 2 * PV_L])
    w_in = din("w_in", [2, D, INC])
    w_attn_o = din("w_attn_o", [2, AW, D])
    w_conv_o = din("w_conv_o", [2, D, D])
    w_out = din("w_out", [2, D, D])
    if use_ffn:
        ffn_w1 = din("ffn_w1", [1, D, DFF])
        ffn_w3 = din("ffn_w3", [1, D, DFF])
        ffn_w2 = din("ffn_w2", [1, DFF, D])
    if use_moe:
        router = din("router", [128, NC_ * NE])
        moe_w1 = din("moe_w1", [1, NE, D, DFF])
        moe_w3 = din("moe_w3", [1, NE, D, DFF])
        moe_w2 = din("moe_w2", [1, NE, DFF, D])
    yT_out = nc.dram_tensor("yT", [D, EXT - OWN0], F32, kind="ExternalOutput").ap()

    qT_d = dscr("qT_d", [AW, EXT], BF16)
    kT_d = dscr("kT_d", [AW, EXT], BF16)
    v_d = dscr("v_d", [EXT, AW], BF16)
    uT_d = dscr("uT_d", [D, EXT], BF16)
    gt_d = dscr("gt_d", [2 * D, EXT], F32)
    oT_d = dscr("oT_d", [AW, EXT], BF16)
    zT_d = dscr("zT_d", [D, EXT], BF16)
    x1T_d = dscr("x1T_d", [D, EXT], F32)
    x2T_d = dscr("x2T_d", [D, EXT], F32)
    pre_list = []
    if use_ffn:
        ffn_w1b = nc.dram_tensor("ffn_w1b", [D, DFF], BF16, kind="Internal").ap()
        ffn_w3b = nc.dram_tensor("ffn_w3b", [D, DFF], BF16, kind="Internal").ap()
        ffn_w2b = nc.dram_tensor("ffn_w2b", [DFF, D], BF16, kind="Internal").ap()
        for dst, srcw, rows in ((ffn_w1b, ffn_w1[0], D), (ffn_w3b, ffn_w3[0], D), (ffn_w2b, ffn_w2[0], DFF)):
            step = rows // 8
            for r0 in range(0, rows, step):
                pre_list.append((dst[r0:r0 + step, :], srcw[r0:r0 + step, :]))
    n_pre_ffn = len(pre_list)
    if use_moe:
        moe_w1b = nc.dram_tensor("moe_w1b", [NE, D, DFF], BF16, kind="Internal").ap()
        moe_w3b = nc.dram_tensor("moe_w3b", [NE, D, DFF], BF16, kind="Internal").ap()
        moe_w2b = nc.dram_tensor("moe_w2b", [NE, DFF, D], BF16, kind="Internal").ap()
        for ex in range(NE):
            for dst, srcw, rows in ((moe_w1b[ex], moe_w1[0, ex], D), (moe_w3b[ex], moe_w3[0, ex], D), (moe_w2b[ex], moe_w2[0, ex], DFF)):
                step = rows // 8
                for r0 in range(0, rows, step):
                    pre_list.append((dst[r0:r0 + step, :], srcw[r0:r0 + step, :]))
    B_pre = Buf(keep=True)
    pre_state = [0, 0]
    PRE_EVERY = 5

    def precast_issue(upto):
        while pre_state[0] < min(upto, len(pre_list)):
            dst, srcw = pre_list[pre_state[0]]
            pre_state[0] += 1
            P.dma("pool", lambda e, dst=dst, srcw=srcw: e.dma_start(out=dst, in_=srcw), B_pre)

    def precast_tick():
        pre_state[1] += 1
        if pre_state[1] % PRE_EVERY == 0:
            precast_issue(pre_state[0] + 1)

    if debug:
        dbg_g = dscr("dbg_g", [128, NE * 512], F32)
        dbg_y = dscr("dbg_y", [128, NC_ * 512], F32)

    cst = P.sb("cst", [128, CSTW], F32)
    cstb = P.sb("cstb", [128, CSTW], BF16)
    pvec = P.sb("pvec", [128, 2 * PV_L], F32)
    hmask = P.sb("hmask", [128, 1], F32)
    maskH = P.sb("maskH", [128, 256], F32)
    B_cst = Buf(keep=True)
    P.dma("sp", lambda e: e.dma_start(out=cst[:], in_=cst_in), B_cst, writes=[B_cst])
    P.dma("sp", lambda e: e.dma_start(out=pvec[:], in_=pvec_in), B_cst, writes=[B_cst])
    P.dma("sp", lambda e: e.dma_start(out=hmask[:], in_=hmask_in), B_cst, writes=[B_cst])
    P.op("dve", lambda e: e.tensor_copy(out=cstb[:], in_=cst[:]), reads=[B_cst], writes=[B_cst])
    P.op("dve", lambda e: e.tensor_copy(out=maskH[:], in_=cst[:, 384:640]), reads=[B_cst], writes=[B_cst])
    P.op("dve", lambda e: e.tensor_scalar(out=maskH[:, 0:128], in0=cst[:, 384:512], scalar1=hmask[:, 0:1],
                                          scalar2=None, op0=ALU.mult), reads=[B_cst], writes=[B_cst])
    ones_f = cst[:, 0:128]
    ident_f = cst[:, 128:256]
    rmat_f = cst[:, 256:384]
    mask_f = cst[:, 384:640]
    ones_b = cstb[:, 0:128]
    ident_b = cstb[:, 128:256]
    tri_b = cstb[:, 640:768]
    rmat_b = cstb[:, 256:384]
    iota_f = cst[:, 768:1024]
    P.barrier()
    mask2 = P.sb("mask2", [128, 512], F32)
    maskH2 = P.sb("maskH2", [128, 512], F32)
    for hh in range(2):
        P.op("dve", lambda e, hh=hh: e.tensor_copy(out=mask2[:, hh * 256:(hh + 1) * 256], in_=cst[:, 384:640]),
             reads=[B_cst], writes=[B_cst])
        P.op("dve", lambda e, hh=hh: e.tensor_copy(out=maskH2[:, hh * 256:(hh + 1) * 256], in_=maskH[:]),
             reads=[B_cst], writes=[B_cst])
    P.barrier()
    P.persist()

    PS = Rot(P, "ps", 5, [128, 512], F32, psum=True)
    PSA = Rot(P, "psa", 3, [128, 512], F32, psum=True)

    class WGPool:
        def __init__(self, n, elems):
            self.t = [P.sb("wg%d" % i, [128, elems], BF16) for i in range(n)]
            self.b = [Buf() for _ in range(n)]
            self.i = 0
            self.n = n

        def next(self):
            i = self.i
            self.i = (i + 1) % self.n
            return self.t[i], self.b[i]

    WGH = [None]

    def load_granule(wsrc2d, kc, c0, ncols):
        t, b = WGH[0].next()
        view = t[:, 0:kc * ncols].rearrange("p (k n) -> p k n", k=kc)
        src = wsrc2d[:, c0:c0 + ncols].rearrange("(k p) n -> p k n", p=128)
        P.dma("pool", lambda e: e.dma_start(out=view, in_=src), b, writes=[b])
        precast_tick()
        return view, b

    def phase1(l, src_T, t_part0, t_full0):
        pv0 = l * PV_L
        TB = 2048
        WGH[0] = WGPool(4, 16 * 512)
        hT = P.sb("p1_hT", [128, NC_, TB], BF16)
        B_hT = Buf()
        XIN = Rot(P, "p1_xin", 3, [128, 512], F32)
        SQ = Rot(P, "p1_sq", 2, [128, 512], BF16)
        rstd = P.sb("p1_rstd", [128, TB], F32)
        B_rstd = Buf()
        cs_t = P.sb("p1_cos", [128, TB], F32)
        sn_t = P.sb("p1_sin", [128, TB], F32)
        B_cs = Buf()
        TMP = Rot(P, "p1_tmp", 7, [128, 512], F32)
        STG = Rot(P, "p1_stg", 6, [128, 512], BF16)
        STF = Rot(P, "p1_stf", 3, [128, 512], F32)
        wl = w_in[l]

        for t0 in range(t_part0, EXT, TB):
            if dbg_blocks is not None and t0 not in dbg_blocks:
                continue
            full = t0 >= t_full0
            nsb = TB // 512
            P.dma("sp", lambda e, t0=t0: e.dma_start(out=cs_t[:], in_=cos_in[:, t0:t0 + TB]), B_cs, writes=[B_cs])
            P.dma("sp", lambda e, t0=t0: e.dma_start(out=sn_t[:], in_=sin_in[:, t0:t0 + TB]), B_cs, writes=[B_cs])
            for sb in range(nsb):
                ts = t0 + sb * 512
                pss, bss = PSA.next()
                for c in range(NC_):
                    xt, bx = XIN.next()
                    P.dma("sp", lambda e, xt=xt, c=c, ts=ts: e.dma_start(
                        out=xt[:], in_=src_T[c * 128:(c + 1) * 128, ts:ts + 512]), bx, writes=[bx])
                    sq, bq = SQ.next()
                    P.op("act", lambda e, sq=sq, xt=xt: e.activation(out=sq[:], in_=xt[:], func=AF.Square),
                         reads=[bx], writes=[bq])
                    P.op("pe", lambda e, pss=pss, sq=sq, c=c: e.matmul(pss[:], lhsT=ones_b, rhs=sq[:],
                                                                      start=(c == 0), stop=(c == NC_ - 1)),
                         reads=[bq], writes=[bss])
                tm, btm = TMP.next()
                P.op("act", lambda e, tm=tm, pss=pss: e.activation(out=tm[:], in_=pss[:], func=AF.Ln,
                                                                    scale=1.0 / D, bias=EPS),
                     reads=[bss], writes=[btm])
                P.op("act", lambda e, tm=tm, sb=sb: e.activation(out=rstd[:, sb * 512:(sb + 1) * 512], in_=tm[:],
                                                                 func=AF.Exp, scale=-0.5),
                     reads=[btm], writes=[B_rstd])
                for c in range(NC_):
                    xt, bx = XIN.next()
                    P.dma("sp", lambda e, xt=xt, c=c, ts=ts: e.dma_start(
                        out=xt[:], in_=src_T[c * 128:(c + 1) * 128, ts:ts + 512]), bx, writes=[bx])
                    P.op("dve", lambda e, xt=xt, c=c, sb=sb: e.scalar_tensor_tensor(
                        out=hT[:, c, sb * 512:(sb + 1) * 512], in0=xt[:], scalar=pvec[:, pv0 + PV_NMIX + c:pv0 + PV_NMIX + c + 1],
                        in1=rstd[:, sb * 512:(sb + 1) * 512], op0=ALU.mult, op1=ALU.mult),
                         reads=[bx, B_rstd], writes=[B_hT])

            tasks = []
            if full:
                for j in range(3):
                    tasks.append(("qk", 0, j))
            for j in range(3):
                tasks.append(("qk", 1, j))
            for j in range(3):
                tasks.append(("v", j))
            for j in range(4):
                tasks.append(("cv", j))
            if full:
                for j in range(8):
                    tasks.append(("gate", j))

            def cols_of(task):
                if task[0] == "qk":
                    return [task[1] * AW + task[2] * 512]
                if task[0] == "v":
                    return [2 * AW + task[1] * 512]
                if task[0] == "cv":
                    return [3 * AW + task[1] * 512, 3 * AW + D + task[1] * 512]
                return [3 * AW + 2 * D + task[1] * 512]

            for task in tasks:
                grs = [load_granule(wl, NC_, c0, 512) for c0 in cols_of(task)]
                kind = task[0]
                trim = (not full) and (kind == "cv" or task[-1] < 2)
                if kind == "v":
                    wv, bw = grs[0]
                    for tile in (range(TB // 128 - 4, TB // 128) if trim else range(TB // 128)):
                        pt, bp = PS.next()
                        for k in range(NC_):
                            P.op("pe", lambda e, pt=pt, k=k, tile=tile, wv=wv: e.matmul(
                                pt[:], lhsT=hT[:, k, tile * 128:(tile + 1) * 128], rhs=wv[:, k, :],
                                start=(k == 0), stop=(k == NC_ - 1)),
                                 reads=[B_hT, bw], writes=[bp], signal=(k == NC_ - 1))
                        st, bs = STG.next()
                        P.op("act", lambda e, st=st, pt=pt: e.activation(out=st[:], in_=pt[:], func=AF.Copy),
                             reads=[bp], writes=[bs])
                        r0 = t0 + tile * 128
                        cc = task[1] * 512
                        P.dma("sp", lambda e, st=st, r0=r0, cc=cc: e.dma_start(
                            out=v_d[r0:r0 + 128, cc:cc + 512], in_=st[:]), bs, reads=[bs])
                    continue
                for m in range(4):
                    for sb in ([nsb - 1] if trim else range(nsb)):
                        ts = t0 + sb * 512
                        tsl = slice(sb * 512, (sb + 1) * 512)
                        pts = []
                        for (wv, bw) in grs:
                            pt, bp = PS.next()
                            for k in range(NC_):
                                P.op("pe", lambda e, pt=pt, k=k, wv=wv, m=m, tsl=tsl: e.matmul(
                                    pt[:], lhsT=wv[:, k, m * 128:(m + 1) * 128], rhs=hT[:, k, tsl],
                                    start=(k == 0), stop=(k == NC_ - 1)),
                                     reads=[B_hT, bw], writes=[bp], signal=(k == NC_ - 1))
                            pts.append((pt, bp))
                        if kind == "qk":
                            isk = task[1]
                            head = task[2] * 4 + m
                            gcol = pv0 + (PV_KN if isk else PV_QN)
                            p0, b0 = pts[0]
                            qs, bqs = TMP.next()
                            sq, bsq = STG.next()
                            qb, bqb = STG.next()
                            P.op("act", lambda e, qs=qs, p0=p0, gcol=gcol: e.activation(
                                out=qs[:], in_=p0[:], func=AF.Copy, scale=pvec[:, gcol:gcol + 1]),
                                 reads=[b0, B_cst], writes=[bqs])
                            P.op("act", lambda e, qb=qb, p0=p0, gcol=gcol: e.activation(
                                out=qb[:], in_=p0[:], func=AF.Copy, scale=pvec[:, gcol:gcol + 1]),
                                 reads=[b0, B_cst], writes=[bqb])
                            P.op("act", lambda e, sq=sq, p0=p0: e.activation(out=sq[:], in_=p0[:], func=AF.Square),
                                 reads=[b0], writes=[bsq])
                            p1, b1 = PS.next()
                            p2, b2 = PS.next()
                            P.op("pe", lambda e, p1=p1, sq=sq: e.matmul(p1[:], lhsT=ones_b, rhs=sq[:], start=True, stop=True),
                                 reads=[bsq], writes=[b1])
                            P.op("pe", lambda e, p2=p2, qb=qb: e.matmul(p2[:], lhsT=rmat_b, rhs=qb[:], start=True, stop=True),
                                 reads=[bqb], writes=[b2])
                            ln, bln = TMP.next()
                            rs, brs = TMP.next()
                            P.op("act", lambda e, ln=ln, p1=p1: e.activation(out=ln[:], in_=p1[:], func=AF.Ln,
                                                                              scale=1.0 / HD, bias=EPS),
                                 reads=[b1], writes=[bln])
                            P.op("act", lambda e, rs=rs, ln=ln: e.activation(out=rs[:], in_=ln[:], func=AF.Exp, scale=-0.5),
                                 reads=[bln], writes=[brs])
                            t1, bt1 = TMP.next()
                            t2, bt2 = TMP.next()
                            P.op("dve", lambda e, t1=t1, qs=qs, tsl=tsl: e.tensor_tensor(
                                out=t1[:], in0=qs[:], in1=cs_t[:, tsl], op=ALU.mult), reads=[bqs, B_cs], writes=[bt1])
                            P.op("dve", lambda e, t2=t2, p2=p2, tsl=tsl: e.tensor_tensor(
                                out=t2[:], in0=p2[:], in1=sn_t[:, tsl], op=ALU.mult), reads=[b2, B_cs], writes=[bt2])
                            P.op("dve", lambda e, t1=t1, t2=t2: e.tensor_tensor(
                                out=t1[:], in0=t1[:], in1=t2[:], op=ALU.add), reads=[bt1, bt2], writes=[bt1])
                            st, bs = STG.next()
                            P.op("dve", lambda e, st=st, t1=t1, rs=rs: e.tensor_tensor(
                                out=st[:], in0=t1[:], in1=rs[:], op=ALU.mult), reads=[bt1, brs], writes=[bs])
                            dst = kT_d if isk else qT_d
                            P.dma("sp", lambda e, st=st, dst=dst, head=head, ts=ts: e.dma_start(
                                out=dst[head * 128:(head + 1) * 128, ts:ts + 512], in_=st[:]), bs, reads=[bs])
                        elif kind == "cv":
                            (pv_, bv_), (pg_, bg_) = pts
                            sg, bsg = TMP.next()
                            P.op("act", lambda e, sg=sg, pg_=pg_: e.activation(out=sg[:], in_=pg_[:], func=AF.Sigmoid),
                                 reads=[bg_], writes=[bsg])
                            st, bs = STG.next()
                            P.op("dve", lambda e, st=st, pv_=pv_, sg=sg: e.tensor_tensor(
                                out=st[:], in0=pv_[:], in1=sg[:], op=ALU.mult), reads=[bv_, bsg], writes=[bs])
                            ch = task[1] * 4 + m
                            P.dma("sp", lambda e, st=st, ch=ch, ts=ts: e.dma_start(
                                out=uT_d[ch * 128:(ch + 1) * 128, ts:ts + 512], in_=st[:]), bs, reads=[bs])
                        else:
                            p0, b0 = pts[0]
                            ch = task[1] * 4 + m
                            bcol = pv0 + PV_GB + ch
                            st, bs = STF.next()
                            P.op("act", lambda e, st=st, p0=p0, bcol=bcol: e.activation(
                                out=st[:], in_=p0[:], func=AF.Sigmoid, bias=pvec[:, bcol:bcol + 1]),
                                 reads=[b0, B_cst], writes=[bs])
                            P.dma("sp", lambda e, st=st, ch=ch, ts=ts: e.dma_start(
                                out=gt_d[ch * 128:(ch + 1) * 128, ts:ts + 512], in_=st[:]), bs, reads=[bs])
        P.phase_end()

    def phase2(l, q_start):
        SB = 2048
        qTg = P.sb("p2_q", [128, 4, SB], BF16)
        kTg = P.sb("p2_k", [128, 4, 2 * SB], BF16)
        B_q, B_k = Buf(), Buf()
        oTg = [P.sb("p2_o%d" % g, [128, 4, SB], BF16) for g in range(3)]
        B_o = [Buf() for _ in range(3)]
        Ls = P.sb("p2_L", [128, 4, SB], F32)
        B_L = Buf()
        VT = Rot(P, "p2_v", 2, [128, 17, 512], BF16)
        ET = Rot(P, "p2_e", 3, [128, 512], F32)
        PT = Rot(P, "p2_p", 4, [128, 512], BF16)
        sc = float(HD) ** -0.5
        for q0 in range(q_start, EXT, SB):
            if dbg_range is not None and not (dbg_range[0] <= q0 < dbg_range[1]):
                continue
            for g in range(3):
                d = DIL[g]
                halo = 128 * d
                NB = SB // halo
                P.dma("sp", lambda e, g=g, q0=q0: e.dma_start(
                    out=qTg[:], in_=qT_d[g * 512:(g + 1) * 512, q0:q0 + SB].rearrange("(h p) t -> p h t", p=128)),
                      B_q, writes=[B_q])
                P.dma("sp", lambda e, g=g, q0=q0, halo=halo: e.dma_start(
                    out=kTg[:, :, 0:halo + SB],
                    in_=kT_d[g * 512:(g + 1) * 512, q0 - halo:q0 + SB].rearrange("(h p) t -> p h t", p=128)),
                      B_k, writes=[B_k])
                for r in range(d):
                    vt, bv = VT.next()
                    st = q0 - halo + r
                    P.dma("sp", lambda e, vt=vt, st=st, d=d, NB=NB, g=g: e.dma_start(
                        out=vt[:, 0:NB + 1, :],
                        in_=v_d[st:st + d * ((NB + 1) * 128 - 1) + 1:d, g * 512:(g + 1) * 512].rearrange("(j p) c -> p j c", p=128)),
                          bv, writes=[bv])
                    for nb in range(NB):
                        qb = nb * halo + r
                        qsl = slice(qb, qb + 127 * d + 1, d)
                        kown = slice(halo + qb, halo + qb + 127 * d + 1, d)
                        kprev = slice(qb, qb + 127 * d + 1, d)
                        mk = maskH2 if (q0 == OWN0 and nb == 0) else mask2
                        po, bpo = PSA.next()
                        pl, bpl = PSA.next()
                        for hp in range(2):
                            psb, bps = PS.next()
                            for hh in range(2):
                                hg = hp * 2 + hh
                                for c, ksl in ((0, kprev), (1, kown)):
                                    col = (hh * 2 + c) * 128
                                    P.op("pe", lambda e, psb=psb, col=col, hg=hg, ksl=ksl, qsl=qsl: e.matmul(
                                        psb[:, col:col + 128], lhsT=kTg[:, hg, ksl], rhs=qTg[:, hg, qsl],
                                        start=True, stop=True),
                                         reads=[B_q, B_k], writes=[bps], signal=(hh == 1 and c == 1))
                            et, be = ET.next()
                            P.op("act", lambda e, et=et, psb=psb: e.activation(out=et[:], in_=psb[:], func=AF.Exp, scale=sc),
                                 reads=[bps], writes=[be])
                            pt, bpt = PT.next()
                            P.op("dve", lambda e, pt=pt, et=et, mk=mk: e.tensor_tensor(out=pt[:], in0=et[:], in1=mk[:], op=ALU.mult),
                                 reads=[be, B_cst], writes=[bpt])
                            for hh in range(2):
                                hg = hp * 2 + hh
                                for c in range(2):
                                    col = (hh * 2 + c) * 128
                                    j = nb + c
                                    P.op("pe", lambda e, po=po, hg=hg, vt=vt, j=j, pt=pt, col=col, c=c: e.matmul(
                                        po[:, hg * 128:(hg + 1) * 128], lhsT=vt[:, j, hg * 128:(hg + 1) * 128],
                                        rhs=pt[:, col:col + 128], start=(c == 0), stop=(c == 1)),
                                         reads=[bv, bpt], writes=[bpo], signal=False)
                                for c in range(2):
                                    col = (hh * 2 + c) * 128
                                    P.op("pe", lambda e, pl=pl, hg=hg, pt=pt, col=col, c=c: e.matmul(
                                        pl[:, hg * 128:(hg + 1) * 128], lhsT=ones_b, rhs=pt[:, col:col + 128],
                                        start=(c == 0), stop=(c == 1)),
                                         reads=[bpt, B_cst], writes=[bpl], signal=(hh == 1 and c == 1))
                        P.op("act", lambda e, g=g, po=po, qsl=qsl: e.activation(
                            out=oTg[g][:, :, qsl], in_=po[:].rearrange("p (h t) -> p h t", h=4), func=AF.Copy),
                             reads=[bpo], writes=[B_o[g]])
                        if g == 0:
                            P.op("dve", lambda e, pl=pl, qsl=qsl: e.tensor_copy(
                                out=Ls[:, :, qsl], in_=pl[:].rearrange("p (h t) -> p h t", h=4)),
                                 reads=[bpl], writes=[B_L])
                        else:
                            P.op("dve", lambda e, pl=pl, qsl=qsl: e.tensor_tensor(
                                out=Ls[:, :, qsl], in0=Ls[:, :, qsl], in1=pl[:].rearrange("p (h t) -> p h t", h=4), op=ALU.add),
                                 reads=[bpl, B_L], writes=[B_L])
            Lf = Ls[:].rearrange("p h t -> p (h t)")
            P.op("act", lambda e: e.activation(out=Lf, in_=Lf, func=AF.Ln), reads=[B_L], writes=[B_L])
            P.op("act", lambda e: e.activation(out=Lf, in_=Lf, func=AF.Exp, scale=-1.0), reads=[B_L], writes=[B_L])
            for g in range(3):
                P.op("dve", lambda e, g=g: e.tensor_tensor(out=oTg[g][:], in0=oTg[g][:], in1=Ls[:], op=ALU.mult),
                     reads=[B_o[g], B_L], writes=[B_o[g]])
                P.dma("sp", lambda e, g=g, q0=q0: e.dma_start(
                    out=oT_d[g * 512:(g + 1) * 512, q0:q0 + SB].rearrange("(h p) t -> p h t", p=128), in_=oTg[g][:]),
                      B_o[g], reads=[B_o[g]])
        P.phase_end()

    def phase3(l, t_start):
        pv0 = l * PV_L
        dg = P.sb("p3_dg", [128, NC_ * CW * 128], BF16)
        B_dg = Buf()
        cbuf = P.sb("p3_c", [128, NC_, 512], F32)
        B_c = [Buf() for _ in range(NC_)]
        UT = Rot(P, "p3_u", 3, [128, 512 + CW - 1], BF16)
        SQ = Rot(P, "p3_sq", 2, [128, 512], F32)
        TMP = Rot(P, "p3_t", 6, [128, 512], F32)
        STG = Rot(P, "p3_z", 3, [128, 512], BF16)
        for c in range(NC_):
            for j in range(CW):
                o = (c * CW + j) * 128
                col = pv0 + PV_CWT + c * CW + j
                P.op("dve", lambda e, o=o, col=col: e.tensor_scalar(
                    out=dg[:, o:o + 128], in0=ident_f, scalar1=pvec[:, col:col + 1], scalar2=None, op0=ALU.mult),
                     reads=[B_cst], writes=[B_dg])
        for t0 in range(t_start, EXT, 512):
            if dbg_range is not None and not (dbg_range[0] <= t0 < dbg_range[1]):
                continue
            ps1, b1 = PSA.next()
            ps2, b2 = PSA.next()
            for c in range(NC_):
                ut, bu = UT.next()
                P.dma("sp", lambda e, ut=ut, c=c, t0=t0: e.dma_start(
                    out=ut[:], in_=uT_d[c * 128:(c + 1) * 128, t0 - (CW - 1):t0 + 512]), bu, writes=[bu])
                pc, bpc = PS.next()
                for j in range(CW):
                    o = (c * CW + j) * 128
                    P.op("pe", lambda e, pc=pc, o=o, ut=ut, j=j: e.matmul(
                        pc[:], lhsT=dg[:, o:o + 128], rhs=ut[:, j:j + 512], start=(j == 0), stop=(j == CW - 1)),
                         reads=[B_dg, bu], writes=[bpc], signal=(j == CW - 1))
                bcol = pv0 + PV_CB + c
                P.op("act", lambda e, c=c, pc=pc, bcol=bcol: e.activation(
                    out=cbuf[:, c, :], in_=pc[:], func=AF.Identity, bias=pvec[:, bcol:bcol + 1]),
                     reads=[bpc, B_cst], writes=[B_c[c]])
                sq, bq = SQ.next()
                P.op("act", lambda e, sq=sq, c=c: e.activation(out=sq[:], in_=cbuf[:, c, :], func=AF.Square),
                     reads=[B_c[c]], writes=[bq])
                P.op("pe", lambda e, ps1=ps1, c=c: e.matmul(ps1[:], lhsT=ones_f, rhs=cbuf[:, c, :],
                                                          start=(c == 0), stop=(c == NC_ - 1)),
                     reads=[B_c[c], B_cst], writes=[b1])
                P.op("pe", lambda e, ps2=ps2, sq=sq, c=c: e.matmul(ps2[:], lhsT=ones_f, rhs=sq[:],
                                                                   start=(c == 0), stop=(c == NC_ - 1)),
                     reads=[bq, B_cst], writes=[b2])
            mean, bm = TMP.next()
            m2, bm2 = TMP.next()
            var, bvr = TMP.next()
            rs, brs = TMP.next()
            P.op("act", lambda e, mean=mean, ps1=ps1: e.activation(out=mean[:], in_=ps1[:], func=AF.Copy, scale=1.0 / D),
                 reads=[b1], writes=[bm])
            P.op("dve", lambda e, m2=m2, mean=mean: e.tensor_tensor(out=m2[:], in0=mean[:], in1=mean[:], op=ALU.mult),
                 reads=[bm], writes=[bm2])
            P.op("dve", lambda e, var=var, ps2=ps2, m2=m2: e.scalar_tensor_tensor(
                out=var[:], in0=ps2[:], scalar=1.0 / D, in1=m2[:], op0=ALU.mult, op1=ALU.subtract),
                 reads=[b2, bm2], writes=[bvr])
            P.op("act", lambda e, var=var: e.activation(out=var[:], in_=var[:], func=AF.Ln, bias=EPS),
                 reads=[bvr], writes=[bvr])
            P.op("act", lambda e, rs=rs, var=var: e.activation(out=rs[:], in_=var[:], func=AF.Exp, scale=-0.5),
                 reads=[bvr], writes=[brs])
            for c in range(NC_):
                tt, bt = TMP.next() if False else SQ.next()
                P.op("dve", lambda e, tt=tt, c=c, mean=mean: e.tensor_tensor(
                    out=tt[:], in0=cbuf[:, c, :], in1=mean[:], op=ALU.subtract), reads=[B_c[c], bm], writes=[bt])
                P.op("dve", lambda e, tt=tt, rs=rs: e.tensor_tensor(out=tt[:], in0=tt[:], in1=rs[:], op=ALU.mult),
                     reads=[bt, brs], writes=[bt])
                zt, bz = STG.next()
                gcol = pv0 + PV_LG + c
                bcol = pv0 + PV_LB + c
                P.op("act", lambda e, zt=zt, tt=tt, gcol=gcol, bcol=bcol: e.activation(
                    out=zt[:], in_=tt[:], func=AF.Silu, scale=pvec[:, gcol:gcol + 1], bias=pvec[:, bcol:bcol + 1]),
                     reads=[bt, B_cst], writes=[bz])
                P.dma("sp", lambda e, zt=zt, c=c, t0=t0: e.dma_start(
                    out=zT_d[c * 128:(c + 1) * 128, t0:t0 + 512], in_=zt[:]), bz, reads=[bz])
        P.phase_end()

    def phase4(l, src_T, t_start):
        TB = 1024
        nsb = TB // 512
        WGH[0] = WGPool(4, 16 * 512)
        oT = P.sb("p4_o", [128, NH, TB], BF16)
        zT = P.sb("p4_z", [128, NC_, TB], BF16)
        mT = P.sb("p4_m", [128, NC_, TB], BF16)
        B_o, B_z, B_m = Buf(), Buf(), Buf()
        GT = Rot(P, "p4_g", 4, [128, 512], F32)
        XI = Rot(P, "p4_x", 3, [128, 512], F32)
        TMP = Rot(P, "p4_t", 4, [128, 512], F32)
        STF = Rot(P, "p4_s", 3, [128, 512], F32)
        for t0 in range(t_start, EXT, TB):
            if dbg_range is not None and not (dbg_range[0] <= t0 < dbg_range[1]):
                continue
            P.dma("sp", lambda e, t0=t0: e.dma_start(
                out=oT[:], in_=oT_d[:, t0:t0 + TB].rearrange("(h p) t -> p h t", p=128)), B_o, writes=[B_o])
            P.dma("sp", lambda e, t0=t0: e.dma_start(
                out=zT[:], in_=zT_d[:, t0:t0 + TB].rearrange("(h p) t -> p h t", p=128)), B_z, writes=[B_z])
            for nbk in range(4):
                wa, ba = load_granule(w_attn_o[l], NH, nbk * 512, 512)
                wc, bc = load_granule(w_conv_o[l], NC_, nbk * 512, 512)
                for m in range(4):
                    ch = nbk * 4 + m
                    for sb in range(nsb):
                        tsl = slice(sb * 512, (sb + 1) * 512)
                        ts = t0 + sb * 512
                        pa, bpa = PS.next()
                        pc, bpc = PS.next()
                        for k in range(NH):
                            P.op("pe", lambda e, pa=pa, wa=wa, k=k, m=m, tsl=tsl: e.matmul(
                                pa[:], lhsT=wa[:, k, m * 128:(m + 1) * 128], rhs=oT[:, k, tsl],
                                start=(k == 0), stop=(k == NH - 1)), reads=[ba, B_o], writes=[bpa], signal=(k == NH - 1))
                        for k in range(NC_):
                            P.op("pe", lambda e, pc=pc, wc=wc, k=k, m=m, tsl=tsl: e.matmul(
                                pc[:], lhsT=wc[:, k, m * 128:(m + 1) * 128], rhs=zT[:, k, tsl],
                                start=(k == 0), stop=(k == NC_ - 1)), reads=[bc, B_z], writes=[bpc], signal=(k == NC_ - 1))
                        ga, bga = GT.next()
                        gc, bgc = GT.next()
                        P.dma("sp", lambda e, ga=ga, ch=ch, ts=ts: e.dma_start(
                            out=ga[:], in_=gt_d[ch * 128:(ch + 1) * 128, ts:ts + 512]), bga, writes=[bga])
                        P.dma("sp", lambda e, gc=gc, ch=ch, ts=ts: e.dma_start(
                            out=gc[:], in_=gt_d[D + ch * 128:D + (ch + 1) * 128, ts:ts + 512]), bgc, writes=[bgc])
                        t1, bt1 = TMP.next()
                        t2, bt2 = TMP.next()
                        P.op("dve", lambda e, t1=t1, pa=pa, ga=ga: e.tensor_tensor(out=t1[:], in0=pa[:], in1=ga[:], op=ALU.mult),
                             reads=[bpa, bga], writes=[bt1])
                        P.op("dve", lambda e, t2=t2, pc=pc, gc=gc: e.tensor_tensor(out=t2[:], in0=pc[:], in1=gc[:], op=ALU.mult),
                             reads=[bpc, bgc], writes=[bt2])
                        P.op("dve", lambda e, t1=t1, t2=t2, ch=ch, tsl=tsl: e.tensor_tensor(
                            out=mT[:, ch, tsl], in0=t1[:], in1=t2[:], op=ALU.add), reads=[bt1, bt2], writes=[B_m])
            for nbk in range(4):
                wo, bo = load_granule(w_out[l], NC_, nbk * 512, 512)
                for m in range(4):
                    ch = nbk * 4 + m
                    for sb in range(nsb):
                        tsl = slice(sb * 512, (sb + 1) * 512)
                        ts = t0 + sb * 512
                        po, bpo = PS.next()
                        for k in range(NC_):
                            P.op("pe", lambda e, po=po, wo=wo, k=k, m=m, tsl=tsl: e.matmul(
                                po[:], lhsT=wo[:, k, m * 128:(m + 1) * 128], rhs=mT[:, k, tsl],
                                start=(k == 0), stop=(k == NC_ - 1)), reads=[bo, B_m], writes=[bpo], signal=(k == NC_ - 1))
                        xi, bxi = XI.next()
                        P.dma("sp", lambda e, xi=xi, ch=ch, ts=ts: e.dma_start(
                            out=xi[:], in_=src_T[ch * 128:(ch + 1) * 128, ts:ts + 512]), bxi, writes=[bxi])
                        st, bs = STF.next()
                        P.op("dve", lambda e, st=st, po=po, xi=xi: e.tensor_tensor(out=st[:], in0=po[:], in1=xi[:], op=ALU.add),
                             reads=[bpo, bxi], writes=[bs])
                        P.dma("sp", lambda e, st=st, ch=ch, ts=ts: e.dma_start(
                            out=x1T_d[ch * 128:(ch + 1) * 128, ts:ts + 512], in_=st[:]), bs, reads=[bs])
        P.phase_end()

    def ffn_norm(l, t0, TB, hT, B_hT, XI, SQ, TMP, rstd, B_rstd, extra=None):
        pv0 = l * PV_L
        for sb in range(TB // 512):
            ts = t0 + sb * 512
            tsl = slice(sb * 512, (sb + 1) * 512)
            pss, bss = PSA.next()
            for c in range(NC_):
                xt, bx = XI.next()
                P.dma("sp", lambda e, xt=xt, c=c, ts=ts: e.dma_start(
                    out=xt[:], in_=x1T_d[c * 128:(c + 1) * 128, ts:ts + 512]), bx, writes=[bx])
                sq, bq = SQ.next()
                P.op("act", lambda e, sq=sq, xt=xt: e.activation(out=sq[:], in_=xt[:], func=AF.Square),
                     reads=[bx], writes=[bq])
                P.op("pe", lambda e, pss=pss, sq=sq, c=c: e.matmul(pss[:], lhsT=ones_b, rhs=sq[:],
                                                                  start=(c == 0), stop=(c == NC_ - 1)),
                     reads=[bq, B_cst], writes=[bss])
            tm, btm = TMP.next()
            P.op("act", lambda e, tm=tm, pss=pss: e.activation(out=tm[:], in_=pss[:], func=AF.Ln, scale=1.0 / D, bias=EPS),
                 reads=[bss], writes=[btm])
            P.op("act", lambda e, tm=tm, tsl=tsl: e.activation(out=rstd[:, tsl], in_=tm[:], func=AF.Exp, scale=-0.5),
                 reads=[btm], writes=[B_rstd])
            for c in range(NC_):
                xt, bx = XI.next()
                P.dma("sp", lambda e, xt=xt, c=c, ts=ts: e.dma_start(
                    out=xt[:], in_=x1T_d[c * 128:(c + 1) * 128, ts:ts + 512]), bx, writes=[bx])
                gcol = pv0 + PV_NFFN + c
                if extra is None:
                    P.op("dve", lambda e, xt=xt, c=c, tsl=tsl, gcol=gcol: e.scalar_tensor_tensor(
                        out=hT[:, c, tsl], in0=xt[:], scalar=pvec[:, gcol:gcol + 1], in1=rstd[:, tsl],
                        op0=ALU.mult, op1=ALU.mult), reads=[bx, B_rstd, B_cst], writes=[B_hT])
                else:
                    extra(xt, bx, c, sb, tsl, gcol)

    def ffn_expert(w1, w3, w2, TB, hT, B_hT, gT, B_gT, TMP, down_epilogue):
        nsb = TB // 512
        for fb in range(DFF // 256):
            g1, b1 = load_granule(w1, NC_, fb * 256, 256)
            g3, b3 = load_granule(w3, NC_, fb * 256, 256)
            for m in range(2):
                fch = fb * 2 + m
                for sb in range(nsb):
                    tsl = slice(sb * 512, (sb + 1) * 512)
                    pa, bpa = PS.next()
                    pb, bpb = PS.next()
                    for k in range(NC_):
                        P.op("pe", lambda e, pa=pa, g1=g1, k=k, m=m, tsl=tsl: e.matmul(
                            pa[:], lhsT=g1[:, k, m * 128:(m + 1) * 128], rhs=hT[:, k, tsl],
                            start=(k == 0), stop=(k == NC_ - 1)), reads=[b1, B_hT], writes=[bpa], signal=(k == NC_ - 1))
                    for k in range(NC_):
                        P.op("pe", lambda e, pb=pb, g3=g3, k=k, m=m, tsl=tsl: e.matmul(
                            pb[:], lhsT=g3[:, k, m * 128:(m + 1) * 128], rhs=hT[:, k, tsl],
                            start=(k == 0), stop=(k == NC_ - 1)), reads=[b3, B_hT], writes=[bpb], signal=(k == NC_ - 1))
                    sl, bsl = TMP.next()
                    P.op("act", lambda e, sl=sl, pa=pa: e.activation(out=sl[:], in_=pa[:], func=AF.Silu),
                         reads=[bpa], writes=[bsl])
                    P.op("dve", lambda e, sl=sl, pb=pb, fch=fch, tsl=tsl: e.tensor_tensor(
                        out=gT[:, fch, tsl], in0=sl[:], in1=pb[:], op=ALU.mult), reads=[bsl, bpb], writes=[B_gT])
        KQ = DFF // 128 // 4
        for chp in range(NC_ // 2):
            pos = [PS.next() for _ in range(2 * nsb)]
            for qt in range(4):
                wv, bw = load_granule(w2[qt * KQ * 128:(qt + 1) * KQ * 128, :], KQ, chp * 256, 256)
                for m in range(2):
                    for sb in range(nsb):
                        tsl = slice(sb * 512, (sb + 1) * 512)
                        po, bpo = pos[m * nsb + sb]
                        for k in range(KQ):
                            kk = qt * KQ + k
                            P.op("pe", lambda e, po=po, wv=wv, k=k, kk=kk, m=m, tsl=tsl: e.matmul(
                                po[:], lhsT=wv[:, k, m * 128:(m + 1) * 128], rhs=gT[:, kk, tsl],
                                start=(kk == 0), stop=(kk == 4 * KQ - 1)),
                                 reads=[bw, B_gT], writes=[bpo], signal=(k == KQ - 1))
            for m in range(2):
                for sb in range(nsb):
                    tsl = slice(sb * 512, (sb + 1) * 512)
                    po, bpo = pos[m * nsb + sb]
                    down_epilogue(chp * 2 + m, sb, tsl, po, bpo)

    def phase5_dense(l, t_start, dst_T, dst_off):
        TB = 512
        WGH[0] = WGPool(6, 16 * 256)
        hT = P.sb("p5_h", [128, NC_, TB], BF16)
        gT = P.sb("p5_g", [128, DFF // 128, TB], BF16)
        B_hT, B_gT = Buf(), Buf()
        rstd = P.sb("p5_r", [128, TB], F32)
        B_rstd = Buf()
        XI = Rot(P, "p5_x", 3, [128, 512], F32)
        SQ = Rot(P, "p5_q", 2, [128, 512], BF16)
        TMP = Rot(P, "p5_t", 3, [128, 512], F32)
        STF = Rot(P, "p5_s", 3, [128, 512], F32)
        for t0 in range(t_start, EXT, TB):
            if dbg_range is not None and not (dbg_range[0] <= t0 < dbg_range[1]):
                continue
            ffn_norm(l, t0, TB, hT, B_hT, XI, SQ, TMP, rstd, B_rstd)
            halo_blk = (l == 0 and t0 < OWN0)

            def epi(ch, sb, tsl, po, bpo, t0=t0, halo_blk=halo_blk):
                ts = t0 + sb * 512
                xi, bxi = XI.next()
                P.dma("sp", lambda e, xi=xi, ch=ch, ts=ts: e.dma_start(
                    out=xi[:], in_=x1T_d[ch * 128:(ch + 1) * 128, ts:ts + 512]), bxi, writes=[bxi])
                st, bs = STF.next()
                P.op("dve", lambda e, st=st, po=po, xi=xi: e.tensor_tensor(out=st[:], in0=po[:], in1=xi[:], op=ALU.add),
                     reads=[bpo, bxi], writes=[bs])
                if halo_blk:
                    P.op("dve", lambda e, st=st: e.tensor_scalar(out=st[:], in0=st[:], scalar1=hmask[:, 0:1], scalar2=None,
                                                                 op0=ALU.mult), reads=[bs, B_cst], writes=[bs])
                P.dma("sp", lambda e, st=st, ch=ch, ts=ts: e.dma_start(
                    out=dst_T[ch * 128:(ch + 1) * 128, ts - dst_off:ts - dst_off + 512], in_=st[:]), bs, reads=[bs])

            ffn_expert(ffn_w1b, ffn_w3b, ffn_w2b, TB, hT, B_hT, gT, B_gT, TMP, epi)
        P.phase_end()

    def phase5_moe(l, t_start, dst_T, dst_off):
        TB = 512
        WGH[0] = WGPool(4, 16 * 256)
        hT = P.sb("p6_h", [128, NC_, TB], BF16)
        gT = P.sb("p6_g", [128, DFF // 128, TB], BF16)
        yacc = P.sb("p6_y", [128, NC_, TB], F32)
        grep = P.sb("p6_gr", [128, NE, TB], F32)
        rt = P.sb("p6_rt", [128, NC_ * NE], F32)
        B_hT, B_gT, B_y, B_gr, B_rt = Buf(), Buf(), Buf(), Buf(), Buf()
        rstd = P.sb("p6_r", [128, TB], F32)
        B_rstd = Buf()
        XI = Rot(P, "p6_x", 3, [128, 512], F32)
        SQ = Rot(P, "p6_q", 2, [128, 512], BF16)
        TMP = Rot(P, "p6_t", 3, [128, 512], F32)
        STF = Rot(P, "p6_s", 3, [128, 512], F32)
        HF = Rot(P, "p6_hf", 2, [128, 512], F32)
        SM = Rot(P, "p6_sm", 8, [128, 16], F32)
        DG = Rot(P, "p6_dg", 2, [128, 128], F32)
        lgT = P.sb("p6_lgT", [128, 512], F32)
        B_lgT = Buf()
        P.dma("sp", lambda e: e.dma_start(out=rt[:], in_=router), B_rt, writes=[B_rt])
        for t0 in range(t_start, EXT, TB):
            if dbg_range is not None and not (dbg_range[0] <= t0 < dbg_range[1]):
                continue
            plT, bplT = PSA.next()

            def extra(xt, bx, c, sb, tsl, gcol):
                hf, bhf = HF.next()
                P.op("dve", lambda e, hf=hf, xt=xt, gcol=gcol, tsl=tsl: e.scalar_tensor_tensor(
                    out=hf[:], in0=xt[:], scalar=pvec[:, gcol:gcol + 1], in1=rstd[:, tsl],
                    op0=ALU.mult, op1=ALU.mult), reads=[bx, B_rstd, B_cst], writes=[bhf])
                P.op("act", lambda e, hf=hf, c=c, tsl=tsl: e.activation(out=hT[:, c, tsl], in_=hf[:], func=AF.Copy),
                     reads=[bhf], writes=[B_hT])
                P.op("pe", lambda e, hf=hf, c=c: e.matmul(
                    plT[0:NE, :], lhsT=rt[:, c * NE:(c + 1) * NE], rhs=hf[:],
                    start=(c == 0), stop=(c == NC_ - 1)), reads=[bhf, B_rt], writes=[bplT])

            ffn_norm(l, t0, TB, hT, B_hT, XI, SQ, TMP, rstd, B_rstd, extra=extra)
            P.op("act", lambda e: e.activation(out=lgT[0:NE, :], in_=plT[0:NE, :], func=AF.Copy),
                 reads=[bplT], writes=[B_lgT])
            for tile in range(4):
                pl_, bl_ = PS.next()
                P.op("pe", lambda e, pl_=pl_, tile=tile: e.matmul(
                    pl_[:, 0:NE], lhsT=lgT[0:NE, tile * 128:(tile + 1) * 128], rhs=ident_f[0:NE, 0:NE],
                    start=True, stop=True), reads=[B_lgT, B_cst], writes=[bl_])
                lg, blg = SM.next()
                m1, bm1 = SM.next()
                m2, bm2 = SM.next()
                e1, be1 = SM.next()
                e2, be2 = SM.next()
                wv, bwv = SM.next()
                gt8, bg8 = SM.next()
                P.op("act", lambda e, lg=lg, pl_=pl_: e.activation(out=lg[:, 0:NE], in_=pl_[:, 0:NE], func=AF.Copy),
                     reads=[bl_], writes=[blg])
                P.op("dve", lambda e, m1=m1, lg=lg: e.reduce_max(out=m1[:, 0:1], in_=lg[:, 0:NE], axis=mybir.AxisListType.X),
                     reads=[blg], writes=[bm1])
                P.op("dve", lambda e, e1=e1, lg=lg, m1=m1: e.tensor_scalar(
                    out=e1[:, 0:NE], in0=lg[:, 0:NE], scalar1=m1[:, 0:1], scalar2=None, op0=ALU.is_equal),
                     reads=[blg, bm1], writes=[be1])
                P.op("dve", lambda e, e2=e2, e1=e1, lg=lg: e.scalar_tensor_tensor(
                    out=e2[:, 0:NE], in0=e1[:, 0:NE], scalar=-1e30, in1=lg[:, 0:NE], op0=ALU.mult, op1=ALU.add),
                     reads=[be1, blg], writes=[be2])
                P.op("dve", lambda e, m2=m2, e2=e2: e.reduce_max(out=m2[:, 0:1], in_=e2[:, 0:NE], axis=mybir.AxisListType.X),
                     reads=[be2], writes=[bm2])
                P.op("dve", lambda e, e2=e2, m2=m2: e.tensor_scalar(
                    out=e2[:, 0:NE], in0=e2[:, 0:NE], scalar1=m2[:, 0:1], scalar2=None, op0=ALU.is_equal),
                     reads=[be2, bm2], writes=[be2])
                P.op("dve", lambda e, wv=wv, m2=m2, m1=m1: e.tensor_tensor(out=wv[:, 0:1], in0=m2[:, 0:1], in1=m1[:, 0:1], op=ALU.subtract),
                     reads=[bm1, bm2], writes=[bwv])
                P.op("act", lambda e, wv=wv: e.activation(out=wv[:, 1:2], in_=wv[:, 0:1], func=AF.Exp), reads=[bwv], writes=[bwv])
                P.op("dve", lambda e, wv=wv: e.tensor_scalar(out=wv[:, 2:3], in0=wv[:, 1:2], scalar1=1.0, scalar2=None, op0=ALU.add),
                     reads=[bwv], writes=[bwv])
                P.op("dve", lambda e, wv=wv: e.reciprocal(out=wv[:, 3:4], in_=wv[:, 2:3]), reads=[bwv], writes=[bwv])
                P.op("dve", lambda e, wv=wv: e.tensor_scalar(out=wv[:, 4:5], in0=wv[:, 3:4], scalar1=-1.0, scalar2=1.0,
                                                             op0=ALU.mult, op1=ALU.add), reads=[bwv], writes=[bwv])
                P.op("dve", lambda e, gt8=gt8, e1=e1, wv=wv: e.tensor_scalar(
                    out=gt8[:, 0:NE], in0=e1[:, 0:NE], scalar1=wv[:, 3:4], scalar2=None, op0=ALU.mult),
                     reads=[be1, bwv], writes=[bg8])
                P.op("dve", lambda e, gt8=gt8, e2=e2, wv=wv: e.scalar_tensor_tensor(
                    out=gt8[:, 0:NE], in0=e2[:, 0:NE], scalar=wv[:, 4:5], in1=gt8[:, 0:NE], op0=ALU.mult, op1=ALU.add),
                     reads=[be2, bwv, bg8], writes=[bg8])
                for ex in range(NE):
                    dgm, bdg = DG.next()
                    P.op("dve", lambda e, dgm=dgm, gt8=gt8, ex=ex: e.tensor_scalar(
                        out=dgm[:], in0=ident_f, scalar1=gt8[:, ex:ex + 1], scalar2=None, op0=ALU.mult),
                         reads=[bg8, B_cst], writes=[bdg])
                    pr, bpr = PS.next()
                    P.op("pe", lambda e, pr=pr, dgm=dgm: e.matmul(pr[:, 0:128], lhsT=ones_f, rhs=dgm[:], start=True, stop=True),
                         reads=[bdg, B_cst], writes=[bpr])
                    P.op("act", lambda e, pr=pr, ex=ex, tile=tile: e.activation(
                        out=grep[:, ex, tile * 128:(tile + 1) * 128], in_=pr[:, 0:128], func=AF.Copy),
                         reads=[bpr], writes=[B_gr])
            for ex in range(NE):
                def epi(ch, sb, tsl, po, bpo, ex=ex):
                    if ex == 0:
                        P.op("dve", lambda e, po=po, ch=ch, ex=ex: e.tensor_tensor(
                            out=yacc[:, ch, :], in0=po[:], in1=grep[:, ex, :], op=ALU.mult),
                             reads=[bpo, B_gr], writes=[B_y])
                    else:
                        tt, bt = TMP.next()
                        P.op("dve", lambda e, tt=tt, po=po, ex=ex: e.tensor_tensor(
                            out=tt[:], in0=po[:], in1=grep[:, ex, :], op=ALU.mult), reads=[bpo, B_gr], writes=[bt])
                        P.op("dve", lambda e, tt=tt, ch=ch: e.tensor_tensor(
                            out=yacc[:, ch, :], in0=yacc[:, ch, :], in1=tt[:], op=ALU.add), reads=[bt, B_y], writes=[B_y])
                ffn_expert(moe_w1[0, ex], moe_w3[0, ex], moe_w2[0, ex], TB, hT, B_hT, gT, B_gT, TMP, epi)
            if debug:
                P.dma("sp", lambda e: e.dma_start(out=dbg_g, in_=grep[:].rearrange("p e t -> p (e t)")), B_gr, reads=[B_gr])
                P.dma("sp", lambda e: e.dma_start(out=dbg_y, in_=yacc[:].rearrange("p c t -> p (c t)")), B_y, reads=[B_y])
            for ch in range(NC_):
                xi, bxi = XI.next()
                P.dma("sp", lambda e, xi=xi, ch=ch, t0=t0: e.dma_start(
                    out=xi[:], in_=x1T_d[ch * 128:(ch + 1) * 128, t0:t0 + 512]), bxi, writes=[bxi])
                st, bs = STF.next()
                P.op("dve", lambda e, st=st, xi=xi, ch=ch: e.tensor_tensor(out=st[:], in0=yacc[:, ch, :], in1=xi[:], op=ALU.add),
                     reads=[B_y, bxi], writes=[bs])
                P.dma("sp", lambda e, st=st, ch=ch, t0=t0: e.dma_start(
                    out=dst_T[ch * 128:(ch + 1) * 128, t0 - dst_off:t0 - dst_off + 512], in_=st[:]), bs, reads=[bs])
        P.phase_end()

    def phase5_moe_sparse(l, t_start, dst_T, dst_off):
        TB = 512
        C = MOE_C
        SL = ((0, 128), (128, C - 128))
        pv0 = l * PV_L
        WGH[0] = WGPool(4, 16 * 256)
        h_tok = P.sb("p7_ht", [128, 4, D], BF16)
        hTe = P.sb("p7_he", [128, NC_, C], BF16)
        gTe = P.sb("p7_ge", [128, DFF // 128, C], BF16)
        oute = P.sb("p7_oe", [128, 2, D], BF16)
        yacc = P.sb("p7_y", [128, NC_, TB], F32)
        rt = P.sb("p7_rt", [128, NC_ * NE], F32)
        lgT = P.sb("p7_lgT", [128, 512], F32)
        G_all = P.sb("p7_G", [128, 4, NE], F32)
        M_all = P.sb("p7_M", [128, 4, NE], F32)
        Mb_all = P.sb("p7_Mb", [128, 4, NE], BF16)
        R_all = P.sb("p7_R", [128, 4, NE], F32)
        rstd = P.sb("p7_r", [128, TB], F32)
        B_ht, B_he, B_ge, B_oe, B_y, B_rt, B_lgT, B_G, B_R, B_rstd = (Buf() for _ in range(10))
        SEL = Rot(P, "p7_sel", 2, [128, 4, C], BF16)
        SELG = Rot(P, "p7_selg", 2, [128, 4, C], BF16)
        SGT = Rot(P, "p7_sgt", 2, [128, 2, TB], BF16)
        XI = Rot(P, "p7_x", 3, [128, 512], F32)
        SQ = Rot(P, "p7_q", 2, [128, 512], BF16)
        TMP = Rot(P, "p7_t", 3, [128, 512], F32)
        STF = Rot(P, "p7_s", 3, [128, 512], F32)
        HF = Rot(P, "p7_hf", 2, [128, 512], F32)
        HB = Rot(P, "p7_hb", 2, [128, 512], BF16)
        SM = Rot(P, "p7_sm", 8, [128, 16], F32)
        P.dma("sp", lambda e: e.dma_start(out=rt[:], in_=router), B_rt, writes=[B_rt])
        for t0 in range(t_start, EXT, TB):
            if dbg_range is not None and not (dbg_range[0] <= t0 < dbg_range[1]):
                continue
            plT, bplT = PSA.next()

            def extra(xt, bx, c, sb, tsl, gcol):
                hf, bhf = HF.next()
                P.op("dve", lambda e, hf=hf, xt=xt, gcol=gcol, tsl=tsl: e.scalar_tensor_tensor(
                    out=hf[:], in0=xt[:], scalar=pvec[:, gcol:gcol + 1], in1=rstd[:, tsl],
                    op0=ALU.mult, op1=ALU.mult), reads=[bx, B_rstd, B_cst], writes=[bhf])
                P.op("pe", lambda e, hf=hf, c=c: e.matmul(
                    plT[0:NE, :], lhsT=rt[:, c * NE:(c + 1) * NE], rhs=hf[:],
                    start=(c == 0), stop=(c == NC_ - 1)), reads=[bhf, B_rt], writes=[bplT])
                hb, bhb = HB.next()
                P.op("act", lambda e, hb=hb, hf=hf: e.activation(out=hb[:], in_=hf[:], func=AF.Copy),
                     reads=[bhf], writes=[bhb])
                ptr, bptr = PS.next()
                for tile in range(4):
                    P.op("pe", lambda e, ptr=ptr, hb=hb, tile=tile: e.matmul(
                        ptr[:, tile * 128:(tile + 1) * 128], lhsT=hb[:, tile * 128:(tile + 1) * 128], rhs=ident_b,
                        start=True, stop=True), reads=[bhb, B_cst], writes=[bptr], signal=(tile == 3))
                P.op("act", lambda e, ptr=ptr, c=c: e.activation(
                    out=h_tok[:, :, c * 128:(c + 1) * 128], in_=ptr[:].rearrange("p (i f) -> p i f", i=4), func=AF.Copy),
                     reads=[bptr], writes=[B_ht])

            ffn_norm(l, t0, TB, None, None, XI, SQ, TMP, rstd, B_rstd, extra=extra)
            P.op("act", lambda e: e.activation(out=lgT[0:NE, :], in_=plT[0:NE, :], func=AF.Copy),
                 reads=[bplT], writes=[B_lgT])
            for tile in range(4):
                pl_, bl_ = PS.next()
                P.op("pe", lambda e, pl_=pl_, tile=tile: e.matmul(
                    pl_[:, 0:NE], lhsT=lgT[0:NE, tile * 128:(tile + 1) * 128], rhs=ident_f[0:NE, 0:NE],
                    start=True, stop=True), reads=[B_lgT, B_cst], writes=[bl_])
                lg, blg = SM.next()
                m1, bm1 = SM.next()
                m2, bm2 = SM.next()
                e1, be1 = SM.next()
                e2, be2 = SM.next()
                wv, bwv = SM.next()
                P.op("act", lambda e, lg=lg, pl_=pl_: e.activation(out=lg[:, 0:NE], in_=pl_[:, 0:NE], func=AF.Copy),
                     reads=[bl_], writes=[blg])
                P.op("dve", lambda e, m1=m1, lg=lg: e.reduce_max(out=m1[:, 0:1], in_=lg[:, 0:NE], axis=mybir.AxisListType.X),
                     reads=[blg], writes=[bm1])
                P.op("dve", lambda e, e1=e1, lg=lg, m1=m1: e.tensor_scalar(
                    out=e1[:, 0:NE], in0=lg[:, 0:NE], scalar1=m1[:, 0:1], scalar2=None, op0=ALU.is_equal),
                     reads=[blg, bm1], writes=[be1])
                P.op("dve", lambda e, e2=e2, e1=e1, lg=lg: e.scalar_tensor_tensor(
                    out=e2[:, 0:NE], in0=e1[:, 0:NE], scalar=-1e30, in1=lg[:, 0:NE], op0=ALU.mult, op1=ALU.add),
                     reads=[be1, blg], writes=[be2])
                P.op("dve", lambda e, m2=m2, e2=e2: e.reduce_max(out=m2[:, 0:1], in_=e2[:, 0:NE], axis=mybir.AxisListType.X),
                     reads=[be2], writes=[bm2])
                P.op("dve", lambda e, e2=e2, m2=m2: e.tensor_scalar(
                    out=e2[:, 0:NE], in0=e2[:, 0:NE], scalar1=m2[:, 0:1], scalar2=None, op0=ALU.is_equal),
                     reads=[be2, bm2], writes=[be2])
                P.op("dve", lambda e, wv=wv, m2=m2, m1=m1: e.tensor_tensor(out=wv[:, 0:1], in0=m2[:, 0:1], in1=m1[:, 0:1], op=ALU.subtract),
                     reads=[bm1, bm2], writes=[bwv])
                P.op("act", lambda e, wv=wv: e.activation(out=wv[:, 1:2], in_=wv[:, 0:1], func=AF.Exp), reads=[bwv], writes=[bwv])
                P.op("dve", lambda e, wv=wv: e.tensor_scalar(out=wv[:, 2:3], in0=wv[:, 1:2], scalar1=1.0, scalar2=None, op0=ALU.add),
                     reads=[bwv], writes=[bwv])
                P.op("dve", lambda e, wv=wv: e.reciprocal(out=wv[:, 3:4], in_=wv[:, 2:3]), reads=[bwv], writes=[bwv])
                P.op("dve", lambda e, wv=wv: e.tensor_scalar(out=wv[:, 4:5], in0=wv[:, 3:4], scalar1=-1.0, scalar2=1.0,
                                                             op0=ALU.mult, op1=ALU.add), reads=[bwv], writes=[bwv])
                P.op("dve", lambda e, e1=e1, wv=wv, tile=tile: e.tensor_scalar(
                    out=G_all[:, tile, :], in0=e1[:, 0:NE], scalar1=wv[:, 3:4], scalar2=None, op0=ALU.mult),
                     reads=[be1, bwv], writes=[B_G])
                P.op("dve", lambda e, e2=e2, wv=wv, tile=tile: e.scalar_tensor_tensor(
                    out=G_all[:, tile, :], in0=e2[:, 0:NE], scalar=wv[:, 4:5], in1=G_all[:, tile, :], op0=ALU.mult, op1=ALU.add),
                     reads=[be2, bwv, B_G], writes=[B_G])
                P.op("dve", lambda e, e1=e1, e2=e2, tile=tile: e.tensor_tensor(
                    out=M_all[:, tile, :], in0=e1[:, 0:NE], in1=e2[:, 0:NE], op=ALU.add), reads=[be1, be2], writes=[B_G])
                P.op("dve", lambda e, tile=tile: e.tensor_copy(out=Mb_all[:, tile, :], in_=M_all[:, tile, :]),
                     reads=[B_G], writes=[B_G])
            for tile in range(4):
                pr, bpr = PS.next()
                for j in range(tile + 1):
                    P.op("pe", lambda e, pr=pr, j=j, tile=tile: e.matmul(
                        pr[:, 0:NE], lhsT=(tri_b if j == tile else ones_b), rhs=Mb_all[:, j, :],
                        start=(j == 0), stop=(j == tile)), reads=[B_G, B_cst], writes=[bpr], signal=(j == tile))
                P.op("act", lambda e, pr=pr, tile=tile: e.activation(out=R_all[:, tile, :], in_=pr[:, 0:NE], func=AF.Copy),
                     reads=[bpr], writes=[B_R])
            for ex in range(NE):
                sel, bsel = SEL.next()
                selg, bselg = SELG.next()
                for tile in range(4):
                    P.op("dve", lambda e, sel=sel, tile=tile, ex=ex: e.tensor_scalar(
                        out=sel[:, tile, :], in0=iota_f[:, 0:C], scalar1=R_all[:, tile, ex:ex + 1],
                        scalar2=M_all[:, tile, ex:ex + 1], op0=ALU.is_equal, op1=ALU.mult),
                         reads=[B_R, B_G, B_cst], writes=[bsel])
                    P.op("dve", lambda e, selg=selg, tile=tile, ex=ex: e.tensor_scalar(
                        out=selg[:, tile, :], in0=iota_f[:, 0:C], scalar1=R_all[:, tile, ex:ex + 1],
                        scalar2=G_all[:, tile, ex:ex + 1], op0=ALU.is_equal, op1=ALU.mult),
                         reads=[B_R, B_G, B_cst], writes=[bselg])
                for c in range(NC_):
                    pg, bpg = PS.next()
                    for tile in range(4):
                        P.op("pe", lambda e, pg=pg, c=c, tile=tile, sel=sel: e.matmul(
                            pg[:, 0:C], lhsT=h_tok[:, tile, c * 128:(c + 1) * 128], rhs=sel[:, tile, :],
                            start=(tile == 0), stop=(tile == 3)), reads=[B_ht, bsel], writes=[bpg], signal=(tile == 3))
                    P.op("act", lambda e, pg=pg, c=c: e.activation(out=hTe[:, c, :], in_=pg[:, 0:C], func=AF.Copy),
                         reads=[bpg], writes=[B_he])
                w1, w3, w2 = moe_w1b[ex], moe_w3b[ex], moe_w2b[ex]
                for fb in range(DFF // 256):
                    g1, b1 = load_granule(w1, NC_, fb * 256, 256)
                    g3, b3 = load_granule(w3, NC_, fb * 256, 256)
                    for m in range(2):
                        fch = fb * 2 + m
                        pa, bpa = PS.next()
                        pb, bpb = PS.next()
                        for k in range(NC_):
                            P.op("pe", lambda e, pa=pa, g1=g1, k=k, m=m: e.matmul(
                                pa[:, 0:C], lhsT=g1[:, k, m * 128:(m + 1) * 128], rhs=hTe[:, k, :],
                                start=(k == 0), stop=(k == NC_ - 1)), reads=[b1, B_he], writes=[bpa], signal=(k == NC_ - 1))
                        for k in range(NC_):
                            P.op("pe", lambda e, pb=pb, g3=g3, k=k, m=m: e.matmul(
                                pb[:, 0:C], lhsT=g3[:, k, m * 128:(m + 1) * 128], rhs=hTe[:, k, :],
                                start=(k == 0), stop=(k == NC_ - 1)), reads=[b3, B_he], writes=[bpb], signal=(k == NC_ - 1))
                        sl, bsl = TMP.next()
                        P.op("act", lambda e, sl=sl, pa=pa: e.activation(out=sl[:, 0:C], in_=pa[:, 0:C], func=AF.Silu),
                             reads=[bpa], writes=[bsl])
                        P.op("dve", lambda e, sl=sl, pb=pb, fch=fch: e.tensor_tensor(
                            out=gTe[:, fch, :], in0=sl[:, 0:C], in1=pb[:, 0:C], op=ALU.mult), reads=[bsl, bpb], writes=[B_ge])
                KQ = DFF // 128 // 4
                for cb in range(D // 256):
                    pos = [PS.next() for _ in range(2)]
                    for qt in range(4):
                        wv_, bw = load_granule(w2[qt * KQ * 128:(qt + 1) * KQ * 128, :], KQ, cb * 256, 256)
                        for j, (s0, sn) in enumerate(SL):
                            po, bpo = pos[j]
                            for k in range(KQ):
                                kk = qt * KQ + k
                                P.op("pe", lambda e, po=po, wv_=wv_, k=k, kk=kk, s0=s0, sn=sn: e.matmul(
                                    po[0:sn, 0:256], lhsT=gTe[:, kk, s0:s0 + sn], rhs=wv_[:, k, :],
                                    start=(kk == 0), stop=(kk == 4 * KQ - 1)),
                                     reads=[bw, B_ge], writes=[bpo], signal=(k == KQ - 1))
                    for j, (s0, sn) in enumerate(SL):
                        po, bpo = pos[j]
                        P.op("act", lambda e, po=po, j=j, sn=sn, cb=cb: e.activation(
                            out=oute[0:sn, j, cb * 256:(cb + 1) * 256], in_=po[0:sn, 0:256], func=AF.Copy),
                             reads=[bpo], writes=[B_oe])
                sgt, bsgt = SGT.next()
                for j, (s0, sn) in enumerate(SL):
                    pt_, bpt_ = PS.next()
                    for tile in range(4):
                        P.op("pe", lambda e, pt_=pt_, selg=selg, tile=tile, s0=s0, sn=sn: e.matmul(
                            pt_[0:sn, tile * 128:(tile + 1) * 128], lhsT=selg[:, tile, s0:s0 + sn], rhs=ident_b,
                            start=True, stop=True), reads=[bselg, B_cst], writes=[bpt_], signal=(tile == 3))
                    P.op("act", lambda e, pt_=pt_, sgt=sgt, j=j, sn=sn: e.activation(
                        out=sgt[0:sn, j, :], in_=pt_[0:sn, :], func=AF.Copy), reads=[bpt_], writes=[bsgt])
                for ch in range(NC_):
                    pc_, bpc_ = PS.next()
                    for j, (s0, sn) in enumerate(SL):
                        P.op("pe", lambda e, pc_=pc_, j=j, sn=sn, ch=ch, sgt=sgt: e.matmul(
                            pc_[:], lhsT=oute[0:sn, j, ch * 128:(ch + 1) * 128], rhs=sgt[0:sn, j, :],
                            start=(j == 0), stop=(j == 1)), reads=[B_oe, bsgt], writes=[bpc_], signal=(j == 1))
                    if ex == 0:
                        P.op("act", lambda e, pc_=pc_, ch=ch: e.activation(out=yacc[:, ch, :], in_=pc_[:], func=AF.Copy),
                             reads=[bpc_], writes=[B_y])
                    else:
                        P.op("dve", lambda e, pc_=pc_, ch=ch: e.tensor_tensor(
                            out=yacc[:, ch, :], in0=yacc[:, ch, :], in1=pc_[:], op=ALU.add), reads=[bpc_, B_y], writes=[B_y])
            for ch in range(NC_):
                xi, bxi = XI.next()
                P.dma("sp", lambda e, xi=xi, ch=ch, t0=t0: e.dma_start(
                    out=xi[:], in_=x1T_d[ch * 128:(ch + 1) * 128, t0:t0 + 512]), bxi, writes=[bxi])
                st, bs = STF.next()
                P.op("dve", lambda e, st=st, xi=xi, ch=ch: e.tensor_tensor(out=st[:], in0=yacc[:, ch, :], in1=xi[:], op=ALU.add),
                     reads=[B_y, bxi], writes=[bs])
                P.dma("sp", lambda e, st=st, ch=ch, t0=t0: e.dma_start(
                    out=dst_T[ch * 128:(ch + 1) * 128, t0 - dst_off:t0 - dst_off + 512], in_=st[:]), bs, reads=[bs])
        P.phase_end()

    src = xT_in
    for l in layers:
        t_part0 = 0 if l == 0 else HALO0
        t_full0 = HALO0 if l == 0 else OWN0
        if not (dbg_blocks == []):
            phase1(l, src, t_part0, t_full0)
        if stop_after == ("p1", l):
            break
        phase2(l, t_full0)
        if stop_after == ("p2", l):
            break
        phase3(l, t_full0)
        if stop_after == ("p3", l):
            break
        precast_issue(n_pre_ffn if (l == 0 and not dbg_moe_l0) else len(pre_list))
        phase4(l, src, t_full0)
        if stop_after == ("p4", l):
            break
        if l == 0:
            phase5_dense(l, t_full0, x2T_d, 0)
            if dbg_moe_l0:
                (phase5_moe_sparse if sparse else phase5_moe)(l, t_full0, yT_out, OWN0)
        else:
            (phase5_moe_sparse if sparse else phase5_moe)(l, t_full0, yT_out, OWN0)
        src = x2T_d

    P.barrier()
    P.emit()
    es.close()
    return nc, P


def _chunked(v):
    return np.ascontiguousarray(np.asarray(v, np.float32).reshape(-1, 128).T)


def host_consts():
    ones = np.ones((128, 128), np.float32)
    ident = np.eye(128, dtype=np.float32)
    rmat = np.zeros((128, 128), np.float32)
    for e in range(16):
        rmat[e + 16, e] = -1.0
        rmat[e, e + 16] = 1.0
    k = np.arange(128)[:, None]
    q = np.arange(128)[None, :]
    m_prev = (k >= q).astype(np.float32)
    m_own = (k <= q).astype(np.float32)
    tri = (k < q).astype(np.float32)
    iota = np.broadcast_to(np.arange(256, dtype=np.float32)[None, :], (128, 256))
    return np.ascontiguousarray(np.concatenate([ones, ident, rmat, m_prev, m_own, tri, iota], axis=1))


def host_pvec(inp):
    cols = []
    for l in range(2):
        blk = [_chunked(inp["norm_mix"][l]), _chunked(inp["norm_ffn"][l]), _chunked(inp["conv_b"][l]),
               _chunked(inp["conv_ln_g"][l]), _chunked(inp["conv_ln_b"][l]), _chunked(inp["gate_b"][l]),
               np.asarray(inp["q_norm"][l], np.float32).reshape(128, 1),
               np.asarray(inp["k_norm"][l], np.float32).reshape(128, 1)]
        cw = np.asarray(inp["conv_w"][l], np.float32).reshape(CW, D)
        cwt = cw.T.reshape(NC_, 128, CW).transpose(1, 0, 2).reshape(128, NC_ * CW)
        blk.append(cwt)
        cols.append(np.concatenate(blk, axis=1))
    pv = np.concatenate(cols, axis=1)
    assert pv.shape == (128, 2 * PV_L), pv.shape
    return np.ascontiguousarray(pv.astype(np.float32))


def host_core_inputs(inp, core):
    b, half = core // 2, core % 2
    s0 = half * 4096
    x = np.asarray(inp["x"], np.float32)
    t = np.arange(EXT) + (s0 - 4096)
    valid = t >= 0
    xT = np.zeros((D, EXT), np.float32)
    xT[:, valid] = x[b, t[valid], :].T
    hm = np.full((128, 1), 1.0 if half == 1 else 0.0, np.float32)
    inv = np.power(np.float32(500000.0), -np.arange(16, dtype=np.float32) * np.float32(2.0) / np.float32(32.0)).astype(np.float32)
    pos = t.astype(np.float32)
    ang = (pos[None, :] * inv[:, None]).astype(np.float32)
    cosT = np.ones((128, EXT), np.float32)
    sinT = np.zeros((128, EXT), np.float32)
    cosT[0:16] = np.cos(ang)
    cosT[16:32] = np.cos(ang)
    sinT[0:16] = np.sin(ang)
    sinT[16:32] = np.sin(ang)
    return {"xT": xT, "hmask": hm, "cosT": cosT, "sinT": sinT}


def host_shared_inputs(inp, use_ffn=True, use_moe=True):
    sh = {"cst": host_consts(), "pvec": host_pvec(inp)}
    for k in ("w_in", "w_attn_o", "w_conv_o", "w_out"):
        sh[k] = np.ascontiguousarray(np.asarray(inp[k], np.float32))
    if use_ffn:
        for k in ("ffn_w1", "ffn_w3", "ffn_w2"):
            sh[k] = np.ascontiguousarray(np.asarray(inp[k], np.float32))
    if use_moe:
        r = np.asarray(inp["router"], np.float32)[0]
        sh["router"] = np.ascontiguousarray(r.reshape(NC_, 128, NE).transpose(1, 0, 2).reshape(128, NC_ * NE))
        for k in ("moe_w1", "moe_w3", "moe_w2"):
            sh[k] = np.ascontiguousarray(np.asarray(inp[k], np.float32))
    return sh


def kernel(**inputs):
    nc, _ = build_program()
    sh = host_shared_inputs(inputs)
    in_maps = []
    for core in range(8):
        m = dict(sh)
        m.update(host_core_inputs(inputs, core))
        in_maps.append(m)
    res = run_bass_kernel_spmd(nc, in_maps, core_ids=list(range(8)))
    out = np.empty((BATCH, SEQ, D), np.float32)
    for core in range(8):
        b, half = core // 2, core % 2
        out[b, half * 4096:(half + 1) * 4096, :] = np.asarray(res.results[core]["yT"]).T
    return out
```

```python
import numpy as np
from contextlib import ExitStack
import concourse.bass as bass
import concourse.mybir as mybir
from concourse.bass_utils import run_bass_kernel_spmd

F32 = mybir.dt.float32
BF16 = mybir.dt.bfloat16
AF = mybir.ActivationFunctionType
ALU = mybir.AluOpType

D = 2048
NC_ = 16
SEQ = 8192
BATCH = 4
HD = 128
NH = 12
AW = 1536
DFF = 7168
NE = 8
INC = 12800
EXT = 8192
OWN0 = 4096
HALO0 = 2048
EPS = 1e-6
DIL = (1, 4, 16)
CW = 31

PV_NMIX, PV_NFFN, PV_CB, PV_LG, PV_LB, PV_GB, PV_QN, PV_KN, PV_CWT = 0, 16, 32, 48, 64, 80, 112, 113, 114
PV_L = 114 + 16 * CW
CSTW = 640 + 128 + 256
MOE_C = 192


class Buf:
    __slots__ = ("w", "r", "semkey", "keep")

    def __init__(self, keep=False):
        self.w = {}
        self.r = {}
        self.semkey = None
        self.keep = keep


class Prog:
    ENG = ("pe", "act", "dve", "pool", "sp")

    def __init__(self, nc, es):
        self.nc = nc
        self.es = es
        self.q = {e: [] for e in self.ENG}
        self.tr = {e: [] for e in self.ENG}
        self.sem = {}
        self.known = {e: {} for e in self.ENG}
        self.pend = {e: [] for e in self.ENG}
        for e in ("pe", "act", "dve", "pool"):
            self.newsem(e)
        self.ndsem = 0
        self.ninst = 0
        self.free_dsem = []
        self.phase_dsem = []
        self.sb_top = 16512
        self.sb_mark = 16512
        self.nalloc = 0

    def newsem(self, key):
        h = self.es.enter_context(self.nc.semaphore("s_" + str(key)))
        self.sem[key] = [h, 0]

    def sb(self, name, shape, dt):
        nbytes = int(np.prod(shape[1:])) * (4 if dt == F32 else 2)
        off = (self.sb_top + 63) // 64 * 64
        assert off + nbytes <= 229376, ("SBUF overflow", name, off, nbytes)
        self.sb_top = off + nbytes
        self.nalloc += 1
        return self.nc.alloc_sbuf_tensor_at("%s_%d" % (name, self.nalloc), list(shape), dt, offset=off)

    def persist(self):
        self.sb_mark = self.sb_top

    def phase_end(self):
        self.barrier()
        self.sb_top = self.sb_mark
        self.free_dsem.extend(self.phase_dsem)
        self.phase_dsem = []

    def ps(self, name, shape, dt=F32):
        return self.es.enter_context(self.nc.psum_tensor(name, shape, dt))

    def _waits(self, eng, reads, writes, skip_own):
        need = {}
        for b in reads:
            for k, v in b.w.items():
                if need.get(k, 0) < v:
                    need[k] = v
        for b in writes:
            for k, v in b.w.items():
                if need.get(k, 0) < v:
                    need[k] = v
            for k, v in b.r.items():
                if need.get(k, 0) < v:
                    need[k] = v
        kn = self.known[eng]
        for k, v in need.items():
            if skip_own and k == eng:
                continue
            if kn.get(k, 0) < v:
                kn[k] = v
                h = self.sem[k][0]
                self.q[eng].append(lambda e, h=h, v=v: e.wait_ge(h, v))
                self.tr[eng].append(("w", k, v))
                self.ninst += 1

    def op(self, eng, fn, reads=(), writes=(), signal=True):
        self._waits(eng, reads, writes, skip_own=(eng == "pe"))
        s = self.sem[eng]
        val = s[1] + 1
        if signal:
            s[1] = val
            h = s[0]
            self.q[eng].append(lambda e, fn=fn, h=h: fn(e).then_inc(h, 1))
            self.tr[eng].append(("i", eng, 1))
        else:
            self.q[eng].append(fn)
        self.ninst += 1
        for b in reads:
            if b.r.get(eng, 0) < val:
                b.r[eng] = val
        for b in writes:
            if b.w.get(eng, 0) < val:
                b.w[eng] = val

    def dma(self, eng, fn, sbuf, reads=(), writes=()):
        if sbuf.semkey is None:
            if self.free_dsem:
                sbuf.semkey = self.free_dsem.pop()
            else:
                sbuf.semkey = "d%d" % self.ndsem
                self.ndsem += 1
                self.newsem(sbuf.semkey)
            if not sbuf.keep:
                self.phase_dsem.append(sbuf.semkey)
        self._waits(eng, reads, writes, skip_own=False)
        s = self.sem[sbuf.semkey]
        s[1] += 16
        val = s[1]
        h = s[0]
        self.q[eng].append(lambda e, fn=fn, h=h: fn(e).then_inc(h, 16))
        self.tr[eng].append(("i", sbuf.semkey, 16))
        self.ninst += 1
        k = sbuf.semkey
        for b in reads:
            if b.r.get(k, 0) < val:
                b.r[k] = val
        for b in writes:
            if b.w.get(k, 0) < val:
                b.w[k] = val

    def barrier(self):
        for eng in self.ENG:
            kn = self.known[eng]
            for k, (h, v) in self.sem.items():
                if v > 0 and kn.get(k, 0) < v:
                    kn[k] = v
                    self.q[eng].append(lambda e, h=h, v=v: e.wait_ge(h, v))
                    self.tr[eng].append(("w", k, v))

    def simulate(self):
        cnt = {k: 0 for k in self.sem}
        ptr = {e: 0 for e in self.ENG}
        while True:
            prog = False
            for e in self.ENG:
                t = self.tr[e]
                p = ptr[e]
                while p < len(t):
                    kind, k, v = t[p]
                    if kind == "i":
                        cnt[k] += v
                    elif cnt[k] < v:
                        break
                    p += 1
                if p != ptr[e]:
                    prog = True
                    ptr[e] = p
            if not prog:
                break
        bad = {e: (ptr[e], len(self.tr[e]), self.tr[e][ptr[e]], cnt[self.tr[e][ptr[e]][1]])
               for e in self.ENG if ptr[e] < len(self.tr[e])}
        return bad

    def emit(self):
        with self.nc.Block() as block:
            q = self.q

            @block.tensor
            def _(e):
                for f in q["pe"]:
                    f(e)

            @block.scalar
            def _(e):
                for f in q["act"]:
                    f(e)

            @block.vector
            def _(e):
                for f in q["dve"]:
                    f(e)

            @block.gpsimd
            def _(e):
                for f in q["pool"]:
                    f(e)

            @block.sync
            def _(e):
                for f in q["sp"]:
                    f(e)


class Rot:
    def __init__(self, P, name, n, shape, dt, psum=False):
        self.t = [(P.ps if psum else P.sb)("%s%d" % (name, i), shape, dt) for i in range(n)]
        self.b = [Buf() for _ in range(n)]
        self.i = 0
        self.n = n

    def next(self):
        i = self.i
        self.i = (i + 1) % self.n
        return self.t[i], self.b[i]


def build_program(stop_after=None, debug=False, layers=(0, 1), use_ffn=True, use_moe=True, dbg_blocks=None, dbg_range=None, dbg_moe_l0=False, sparse=True):
    nc = bass.Bass("TRN2", target_bir_lowering=False)
    es = ExitStack()
    P = Prog(nc, es)
    kind_s = "ExternalOutput" if debug else "Internal"

    def din(name, shape, dt=F32):
        return nc.dram_tensor(name, list(shape), dt, kind="ExternalInput").ap()

    def dscr(name, shape, dt):
        return nc.dram_tensor(name, list(shape), dt, kind=kind_s).ap()

    xT_in = din("xT", [D, EXT])
    hmask_in = din("hmask", [128, 1])
    cos_in = din("cosT", [128, EXT])
    sin_in = din("sinT", [128, EXT])
    cst_in = din("cst", [128, CSTW])
    pvec_in = din("pvec", [128, 2 * PV_L])
    w_in = din("w_in", [2, D, INC])
    w_attn_o = din("w_attn_o", [2, AW, D])
    w_conv_o = din("w_conv_o", [2, D, D])
    w_out = din("w_out", [2, D, D])
    if use_ffn:
        ffn_w1 = din("ffn_w1", [1, D, DFF])
        ffn_w3 = din("ffn_w3", [1, D, DFF])
        ffn_w2 = din("ffn_w2", [1, DFF, D])
    if use_moe:
        router = din("router", [128, NC_ * NE])
        moe_w1 = din("moe_w1", [1, NE, D, DFF])
        moe_w3 = din("moe_w3", [1, NE, D, DFF])
        moe_w2 = din("moe_w2", [1, NE, DFF, D])
    yT_out = nc.dram_tensor("yT", [D, EXT - OWN0], F32, kind="ExternalOutput").ap()

    qT_d = dscr("qT_d", [AW, EXT], BF16)
    kT_d = dscr("kT_d", [AW, EXT], BF16)
    v_d = dscr("v_d", [EXT, AW], BF16)
    uT_d = dscr("uT_d", [D, EXT], BF16)
    gt_d = dscr("gt_d", [2 * D, EXT], F32)
    oT_d = dscr("oT_d", [AW, EXT], BF16)
    zT_d = dscr("zT_d", [D, EXT], BF16)
    x1T_d = dscr("x1T_d", [D, EXT], F32)
    x2T_d = dscr("x2T_d", [D, EXT], F32)
    pre_list = []
    if use_ffn:
        ffn_w1b = nc.dram_tensor("ffn_w1b", [D, DFF], BF16, kind="Internal").ap()
        ffn_w3b = nc.dram_tensor("ffn_w3b", [D, DFF], BF16, kind="Internal").ap()
        ffn_w2b = nc.dram_tensor("ffn_w2b", [DFF, D], BF16, kind="Internal").ap()
        for dst, srcw, rows in ((ffn_w1b, ffn_w1[0], D), (ffn_w3b, ffn_w3[0], D), (ffn_w2b, ffn_w2[0], DFF)):
            step = rows // 8
            for r0 in range(0, rows, step):
                pre_list.append((dst[r0:r0 + step, :], srcw[r0:r0 + step, :]))
    n_pre_ffn = len(pre_list)
    if use_moe:
        moe_w1b = nc.dram_tensor("moe_w1b", [NE, D, DFF], BF16, kind="Internal").ap()
        moe_w3b = nc.dram_tensor("moe_w3b", [NE, D, DFF], BF16, kind="Internal").ap()
        moe_w2b = nc.dram_tensor("moe_w2b", [NE, DFF, D], BF16, kind="Internal").ap()
        for ex in range(NE):
            for dst, srcw, rows in ((moe_w1b[ex], moe_w1[0, ex], D), (moe_w3b[ex], moe_w3[0, ex], D), (moe_w2b[ex], moe_w2[0, ex], DFF)):
                step = rows // 8
                for r0 in range(0, rows, step):
                    pre_list.append((dst[r0:r0 + step, :], srcw[r0:r0 + step, :]))
    B_pre = Buf(keep=True)
    pre_state = [0, 0]
    PRE_EVERY = 5

    def precast_issue(upto):
        while pre_state[0] < min(upto, len(pre_list)):
            dst, srcw = pre_list[pre_state[0]]
            pre_state[0] += 1
            P.dma("pool", lambda e, dst=dst, srcw=srcw: e.dma_start(out=dst, in_=srcw), B_pre)

    def precast_tick():
        pre_state[1] += 1
        if pre_state[1] % PRE_EVERY == 0:
            precast_issue(pre_state[0] + 1)

    if debug:
        dbg_g = dscr("dbg_g", [128, NE * 512], F32)
        dbg_y = dscr("dbg_y", [128, NC_ * 512], F32)

    cst = P.sb("cst", [128, CSTW], F32)
    cstb = P.sb("cstb", [128, CSTW], BF16)
    pvec = P.sb("pvec", [128, 2 * PV_L], F32)
    hmask = P.sb("hmask", [128, 1], F32)
    maskH = P.sb("maskH", [128, 256], F32)
    B_cst = Buf(keep=True)
    P.dma("sp", lambda e: e.dma_start(out=cst[:], in_=cst_in), B_cst, writes=[B_cst])
    P.dma("sp", lambda e: e.dma_start(out=pvec[:], in_=pvec_in), B_cst, writes=[B_cst])
    P.dma("sp", lambda e: e.dma_start(out=hmask[:], in_=hmask_in), B_cst, writes=[B_cst])
    P.op("dve", lambda e: e.tensor_copy(out=cstb[:], in_=cst[:]), reads=[B_cst], writes=[B_cst])
    P.op("dve", lambda e: e.tensor_copy(out=maskH[:], in_=cst[:, 384:640]), reads=[B_cst], writes=[B_cst])
    P.op("dve", lambda e: e.tensor_scalar(out=maskH[:, 0:128], in0=cst[:, 384:512], scalar1=hmask[:, 0:1],
                                          scalar2=None, op0=ALU.mult), reads=[B_cst], writes=[B_cst])
    ones_f = cst[:, 0:128]
    ident_f = cst[:, 128:256]
    rmat_f = cst[:, 256:384]
    mask_f = cst[:, 384:640]
    ones_b = cstb[:, 0:128]
    ident_b = cstb[:, 128:256]
    tri_b = cstb[:, 640:768]
    rmat_b = cstb[:, 256:384]
    iota_f = cst[:, 768:1024]
    P.barrier()
    mask2 = P.sb("mask2", [128, 512], F32)
    maskH2 = P.sb("maskH2", [128, 512], F32)
    for hh in range(2):
        P.op("dve", lambda e, hh=hh: e.tensor_copy(out=mask2[:, hh * 256:(hh + 1) * 256], in_=cst[:, 384:640]),
             reads=[B_cst], writes=[B_cst])
        P.op("dve", lambda e, hh=hh: e.tensor_copy(out=maskH2[:, hh * 256:(hh + 1) * 256], in_=maskH[:]),
             reads=[B_cst], writes=[B_cst])
    P.barrier()
    P.persist()

    PS = Rot(P, "ps", 5, [128, 512], F32, psum=True)
    PSA = Rot(P, "psa", 3, [128, 512], F32, psum=True)

    class WGPool:
        def __init__(self, n, elems):
            self.t = [P.sb("wg%d" % i, [128, elems], BF16) for i in range(n)]
            self.b = [Buf() for _ in range(n)]
            self.i = 0
            self.n = n

        def next(self):
            i = self.i
            self.i = (i + 1) % self.n
            return self.t[i], self.b[i]

    WGH = [None]

    def load_granule(wsrc2d, kc, c0, ncols):
        t, b = WGH[0].next()
        view = t[:, 0:kc * ncols].rearrange("p (k n) -> p k n", k=kc)
        src = wsrc2d[:, c0:c0 + ncols].rearrange("(k p) n -> p k n", p=128)
        P.dma("pool", lambda e: e.dma_start(out=view, in_=src), b, writes=[b])
        precast_tick()
        return view, b

    def phase1(l, src_T, t_part0, t_full0):
        pv0 = l * PV_L
        TB = 2048
        WGH[0] = WGPool(4, 16 * 512)
        hT = P.sb("p1_hT", [128, NC_, TB], BF16)
        B_hT = Buf()
        XIN = Rot(P, "p1_xin", 3, [128, 512], F32)
        SQ = Rot(P, "p1_sq", 2, [128, 512], BF16)
        rstd = P.sb("p1_rstd", [128, TB], F32)
        B_rstd = Buf()
        cs_t = P.sb("p1_cos", [128, TB], F32)
        sn_t = P.sb("p1_sin", [128, TB], F32)
        B_cs = Buf()
        TMP = Rot(P, "p1_tmp", 7, [128, 512], F32)
        STG = Rot(P, "p1_stg", 6, [128, 512], BF16)
        STF = Rot(P, "p1_stf", 3, [128, 512], F32)
        wl = w_in[l]

        for t0 in range(t_part0, EXT, TB):
            if dbg_blocks is not None and t0 not in dbg_blocks:
                continue
            full = t0 >= t_full0
            nsb = TB // 512
            P.dma("sp", lambda e, t0=t0: e.dma_start(out=cs_t[:], in_=cos_in[:, t0:t0 + TB]), B_cs, writes=[B_cs])
            P.dma("sp", lambda e, t0=t0: e.dma_start(out=sn_t[:], in_=sin_in[:, t0:t0 + TB]), B_cs, writes=[B_cs])
            for sb in range(nsb):
                ts = t0 + sb * 512
                pss, bss = PSA.next()
                for c in range(NC_):
                    xt, bx = XIN.next()
                    P.dma("sp", lambda e, xt=xt, c=c, ts=ts: e.dma_start(
                        out=xt[:], in_=src_T[c * 128:(c + 1) * 128, ts:ts + 512]), bx, writes=[bx])
                    sq, bq = SQ.next()
                    P.op("act", lambda e, sq=sq, xt=xt: e.activation(out=sq[:], in_=xt[:], func=AF.Square),
                         reads=[bx], writes=[bq])
                    P.op("pe", lambda e, pss=pss, sq=sq, c=c: e.matmul(pss[:], lhsT=ones_b, rhs=sq[:],
                                                                      start=(c == 0), stop=(c == NC_ - 1)),
                         reads=[bq], writes=[bss])
                tm, btm = TMP.next()
                P.op("act", lambda e, tm=tm, pss=pss: e.activation(out=tm[:], in_=pss[:], func=AF.Ln,
                                                                    scale=1.0 / D, bias=EPS),
                     reads=[bss], writes=[btm])
                P.op("act", lambda e, tm=tm, sb=sb: e.activation(out=rstd[:, sb * 512:(sb + 1) * 512], in_=tm[:],
                                                                 func=AF.Exp, scale=-0.5),
                     reads=[btm], writes=[B_rstd])
                for c in range(NC_):
                    xt, bx = XIN.next()
                    P.dma("sp", lambda e, xt=xt, c=c, ts=ts: e.dma_start(
                        out=xt[:], in_=src_T[c * 128:(c + 1) * 128, ts:ts + 512]), bx, writes=[bx])
                    P.op("dve", lambda e, xt=xt, c=c, sb=sb: e.scalar_tensor_tensor(
                        out=hT[:, c, sb * 512:(sb + 1) * 512], in0=xt[:], scalar=pvec[:, pv0 + PV_NMIX + c:pv0 + PV_NMIX + c + 1],
                        in1=rstd[:, sb * 512:(sb + 1) * 512], op0=ALU.mult, op1=ALU.mult),
                         reads=[bx, B_rstd], writes=[B_hT])

            tasks = []
            if full:
                for j in range(3):
                    tasks.append(("qk", 0, j))
            for j in range(3):
                tasks.append(("qk", 1, j))
            for j in range(3):
                tasks.append(("v", j))
            for j in range(4):
                tasks.append(("cv", j))
            if full:
                for j in range(8):
                    tasks.append(("gate", j))

            def cols_of(task):
                if task[0] == "qk":
                    return [task[1] * AW + task[2] * 512]
                if task[0] == "v":
                    return [2 * AW + task[1] * 512]
                if task[0] == "cv":
                    return [3 * AW + task[1] * 512, 3 * AW + D + task[1] * 512]
                return [3 * AW + 2 * D + task[1] * 512]

            for task in tasks:
                grs = [load_granule(wl, NC_, c0, 512) for c0 in cols_of(task)]
                kind = task[0]
                trim = (not full) and (kind == "cv" or task[-1] < 2)
                if kind == "v":
                    wv, bw = grs[0]
                    for tile in (range(TB // 128 - 4, TB // 128) if trim else range(TB // 128)):
                        pt, bp = PS.next()
                        for k in range(NC_):
                            P.op("pe", lambda e, pt=pt, k=k, tile=tile, wv=wv: e.matmul(
                                pt[:], lhsT=hT[:, k, tile * 128:(tile + 1) * 128], rhs=wv[:, k, :],
                                start=(k == 0), stop=(k == NC_ - 1)),
                                 reads=[B_hT, bw], writes=[bp], signal=(k == NC_ - 1))
                        st, bs = STG.next()
                        P.op("act", lambda e, st=st, pt=pt: e.activation(out=st[:], in_=pt[:], func=AF.Copy),
                             reads=[bp], writes=[bs])
                        r0 = t0 + tile * 128
                        cc = task[1] * 512
                        P.dma("sp", lambda e, st=st, r0=r0, cc=cc: e.dma_start(
                            out=v_d[r0:r0 + 128, cc:cc + 512], in_=st[:]), bs, reads=[bs])
                    continue
                for m in range(4):
                    for sb in ([nsb - 1] if trim else range(nsb)):
                        ts = t0 + sb * 512
                        tsl = slice(sb * 512, (sb + 1) * 512)
                        pts = []
                        for (wv, bw) in grs:
                            pt, bp = PS.next()
                            for k in range(NC_):
                                P.op("pe", lambda e, pt=pt, k=k, wv=wv, m=m, tsl=tsl: e.matmul(
                                    pt[:], lhsT=wv[:, k, m * 128:(m + 1) * 128], rhs=hT[:, k, tsl],
                                    start=(k == 0), stop=(k == NC_ - 1)),
                                     reads=[B_hT, bw], writes=[bp], signal=(k == NC_ - 1))
                            pts.append((pt, bp))
                        if kind == "qk":
                            isk = task[1]
                            head = task[2] * 4 + m
                            gcol = pv0 + (PV_KN if isk else PV_QN)
                            p0, b0 = pts[0]
                            qs, bqs = TMP.next()
                            sq, bsq = STG.next()
                            qb, bqb = STG.next()
                            P.op("act", lambda e, qs=qs, p0=p0, gcol=gcol: e.activation(
                                out=qs[:], in_=p0[:], func=AF.Copy, scale=pvec[:, gcol:gcol + 1]),
                                 reads=[b0, B_cst], writes=[bqs])
                            P.op("act", lambda e, qb=qb, p0=p0, gcol=gcol: e.activation(
                                out=qb[:], in_=p0[:], func=AF.Copy, scale=pvec[:, gcol:gcol + 1]),
                                 reads=[b0, B_cst], writes=[bqb])
                            P.op("act", lambda e, sq=sq, p0=p0: e.activation(out=sq[:], in_=p0[:], func=AF.Square),
                                 reads=[b0], writes=[bsq])
                            p1, b1 = PS.next()
                            p2, b2 = PS.next()
                            P.op("pe", lambda e, p1=p1, sq=sq: e.matmul(p1[:], lhsT=ones_b, rhs=sq[:], start=True, stop=True),
                                 reads=[bsq], writes=[b1])
                            P.op("pe", lambda e, p2=p2, qb=qb: e.matmul(p2[:], lhsT=rmat_b, rhs=qb[:], start=True, stop=True),
                                 reads=[bqb], writes=[b2])
                            ln, bln = TMP.next()
                            rs, brs = TMP.next()
                            P.op("act", lambda e, ln=ln, p1=p1: e.activation(out=ln[:], in_=p1[:], func=AF.Ln,
                                                                              scale=1.0 / HD, bias=EPS),
                                 reads=[b1], writes=[bln])
                            P.op("act", lambda e, rs=rs, ln=ln: e.activation(out=rs[:], in_=ln[:], func=AF.Exp, scale=-0.5),
                                 reads=[bln], writes=[brs])
                            t1, bt1 = TMP.next()
                            t2, bt2 = TMP.next()
                            P.op("dve", lambda e, t1=t1, qs=qs, tsl=tsl: e.tensor_tensor(
                                out=t1[:], in0=qs[:], in1=cs_t[:, tsl], op=ALU.mult), reads=[bqs, B_cs], writes=[bt1])
                            P.op("dve", lambda e, t2=t2, p2=p2, tsl=tsl: e.tensor_tensor(
                                out=t2[:], in0=p2[:], in1=sn_t[:, tsl], op=ALU.mult), reads=[b2, B_cs], writes=[bt2])
                            P.op("dve", lambda e, t1=t1, t2=t2: e.tensor_tensor(
                                out=t1[:], in0=t1[:], in1=t2[:], op=ALU.add), reads=[bt1, bt2], writes=[bt1])
                            st, bs = STG.next()
                            P.op("dve", lambda e, st=st, t1=t1, rs=rs: e.tensor_tensor(
                                out=st[:], in0=t1[:], in1=rs[:], op=ALU.mult), reads=[bt1, brs], writes=[bs])
                            dst = kT_d if isk else qT_d
                            P.dma("sp", lambda e, st=st, dst=dst, head=head, ts=ts: e.dma_start(
                                out=dst[head * 128:(head + 1) * 128, ts:ts + 512], in_=st[:]), bs, reads=[bs])
                        elif kind == "cv":
                            (pv_, bv_), (pg_, bg_) = pts
                            sg, bsg = TMP.next()
                            P.op("act", lambda e, sg=sg, pg_=pg_: e.activation(out=sg[:], in_=pg_[:], func=AF.Sigmoid),
                                 reads=[bg_], writes=[bsg])
                            st, bs = STG.next()
                            P.op("dve", lambda e, st=st, pv_=pv_, sg=sg: e.tensor_tensor(
                                out=st[:], in0=pv_[:], in1=sg[:], op=ALU.mult), reads=[bv_, bsg], writes=[bs])
                            ch = task[1] * 4 + m
                            P.dma("sp", lambda e, st=st, ch=ch, ts=ts: e.dma_start(
                                out=uT_d[ch * 128:(ch + 1) * 128, ts:ts + 512], in_=st[:]), bs, reads=[bs])
                        else:
                            p0, b0 = pts[0]
                            ch = task[1] * 4 + m
                            bcol = pv0 + PV_GB + ch
                            st, bs = STF.next()
                            P.op("act", lambda e, st=st, p0=p0, bcol=bcol: e.activation(
                                out=st[:], in_=p0[:], func=AF.Sigmoid, bias=pvec[:, bcol:bcol + 1]),
                                 reads=[b0, B_cst], writes=[bs])
                            P.dma("sp", lambda e, st=st, ch=ch, ts=ts: e.dma_start(
                                out=gt_d[ch * 128:(ch + 1) * 128, ts:ts + 512], in_=st[:]), bs, reads=[bs])
        P.phase_end()

    def phase2(l, q_start):
        SB = 2048
        qTg = P.sb("p2_q", [128, 4, SB], BF16)
        kTg = P.sb("p2_k", [128, 4, 2 * SB], BF16)
        B_q, B_k = Buf(), Buf()
        oTg = [P.sb("p2_o%d" % g, [128, 4, SB], BF16) for g in range(3)]
        B_o = [Buf() for _ in range(3)]
        Ls = P.sb("p2_L", [128, 4, SB], F32)
        B_L = Buf()
        VT = Rot(P, "p2_v", 2, [128, 17, 512], BF16)
        ET = Rot(P, "p2_e", 3, [128, 512], F32)
        PT = Rot(P, "p2_p", 4, [128, 512], BF16)
        sc = float(HD) ** -0.5

        class RotV:
            def __init__(self, ts):
                self.t = ts
                self.b = [Buf() for _ in ts]
                self.i = 0

            def next(self):
                i = self.i
                self.i = (i + 1) % len(self.t)
                return self.t[i], self.b[i]

        P2S = RotV(PS.t[0:4])
        P2O = RotV([PS.t[4]] + PSA.t[0:3])
        for q0 in range(q_start, EXT, SB):
            if dbg_range is not None and not (dbg_range[0] <= q0 < dbg_range[1]):
                continue
            for g in range(3):
                d = DIL[g]
                halo = 128 * d
                NB = SB // halo
                P.dma("sp", lambda e, g=g, q0=q0: e.dma_start(
                    out=qTg[:], in_=qT_d[g * 512:(g + 1) * 512, q0:q0 + SB].rearrange("(h p) t -> p h t", p=128)),
                      B_q, writes=[B_q])
                P.dma("sp", lambda e, g=g, q0=q0, halo=halo: e.dma_start(
                    out=kTg[:, :, 0:halo + SB],
                    in_=kT_d[g * 512:(g + 1) * 512, q0 - halo:q0 + SB].rearrange("(h p) t -> p h t", p=128)),
                      B_k, writes=[B_k])
                for r in range(d):
                    vt, bv = VT.next()
                    st = q0 - halo + r
                    P.dma("sp", lambda e, vt=vt, st=st, d=d, NB=NB, g=g: e.dma_start(
                        out=vt[:, 0:NB + 1, :],
                        in_=v_d[st:st + d * ((NB + 1) * 128 - 1) + 1:d, g * 512:(g + 1) * 512].rearrange("(j p) c -> p j c", p=128)),
                          bv, writes=[bv])
                    for nb in range(NB):
                        qb = nb * halo + r
                        qsl = slice(qb, qb + 127 * d + 1, d)
                        kown = slice(halo + qb, halo + qb + 127 * d + 1, d)
                        kprev = slice(qb, qb + 127 * d + 1, d)
                        mk = maskH2 if (q0 == OWN0 and nb == 0) else mask2
                        po, bpo = P2O.next()
                        pl, bpl = P2O.next()
                        for hp in range(2):
                            psb, bps = P2S.next()
                            for hh in range(2):
                                hg = hp * 2 + hh
                                for c, ksl in ((0, kprev), (1, kown)):
                                    col = (hh * 2 + c) * 128
                                    P.op("pe", lambda e, psb=psb, col=col, hg=hg, ksl=ksl, qsl=qsl: e.matmul(
                                        psb[:, col:col + 128], lhsT=kTg[:, hg, ksl], rhs=qTg[:, hg, qsl],
                                        start=True, stop=True),
                                         reads=[B_q, B_k], writes=[bps], signal=(hh == 1 and c == 1))
                            et, be = ET.next()
                            P.op("act", lambda e, et=et, psb=psb: e.activation(out=et[:], in_=psb[:], func=AF.Exp, scale=sc),
                                 reads=[bps], writes=[be])
                            pt, bpt = PT.next()
                            P.op("dve", lambda e, pt=pt, et=et, mk=mk: e.tensor_tensor(out=pt[:], in0=et[:], in1=mk[:], op=ALU.mult),
                                 reads=[be, B_cst], writes=[bpt])
                            for hh in range(2):
                                hg = hp * 2 + hh
                                for c in range(2):
                                    col = (hh * 2 + c) * 128
                                    j = nb + c
                                    P.op("pe", lambda e, po=po, hg=hg, vt=vt, j=j, pt=pt, col=col, c=c: e.matmul(
                                        po[:, hg * 128:(hg + 1) * 128], lhsT=vt[:, j, hg * 128:(hg + 1) * 128],
                                        rhs=pt[:, col:col + 128], start=(c == 0), stop=(c == 1)),
                                         reads=[bv, bpt], writes=[bpo], signal=False)
                                for c in range(2):
                                    col = (hh * 2 + c) * 128
                                    P.op("pe", lambda e, pl=pl, hg=hg, pt=pt, col=col, c=c: e.matmul(
                                        pl[:, hg * 128:(hg + 1) * 128], lhsT=ones_b, rhs=pt[:, col:col + 128],
                                        start=(c == 0), stop=(c == 1)),
                                         reads=[bpt, B_cst], writes=[bpl], signal=(hh == 1 and c == 1))
                        P.op("act", lambda e, g=g, po=po, qsl=qsl: e.activation(
                            out=oTg[g][:, :, qsl], in_=po[:].rearrange("p (h t) -> p h t", h=4), func=AF.Copy),
                             reads=[bpo], writes=[B_o[g]])
                        if g == 0:
                            P.op("dve", lambda e, pl=pl, qsl=qsl: e.tensor_copy(
                                out=Ls[:, :, qsl], in_=pl[:].rearrange("p (h t) -> p h t", h=4)),
                                 reads=[bpl], writes=[B_L])
                        else:
                            P.op("dve", lambda e, pl=pl, qsl=qsl: e.tensor_tensor(
                                out=Ls[:, :, qsl], in0=Ls[:, :, qsl], in1=pl[:].rearrange("p (h t) -> p h t", h=4), op=ALU.add),
                                 reads=[bpl, B_L], writes=[B_L])
            Lf = Ls[:].rearrange("p h t -> p (h t)")
            P.op("act", lambda e: e.activation(out=Lf, in_=Lf, func=AF.Ln), reads=[B_L], writes=[B_L])
            P.op("act", lambda e: e.activation(out=Lf, in_=Lf, func=AF.Exp, scale=-1.0), reads=[B_L], writes=[B_L])
            for g in range(3):
                P.op("dve", lambda e, g=g: e.tensor_tensor(out=oTg[g][:], in0=oTg[g][:], in1=Ls[:], op=ALU.mult),
                     reads=[B_o[g], B_L], writes=[B_o[g]])
                P.dma("sp", lambda e, g=g, q0=q0: e.dma_start(
                    out=oT_d[g * 512:(g + 1) * 512, q0:q0 + SB].rearrange("(h p) t -> p h t", p=128), in_=oTg[g][:]),
                      B_o[g], reads=[B_o[g]])
        P.phase_end()

    def phase3(l, t_start):
        pv0 = l * PV_L
        dg = P.sb("p3_dg", [128, NC_ * CW * 128], BF16)
        B_dg = Buf()
        cbuf = P.sb("p3_c", [128, NC_, 512], F32)
        B_c = [Buf() for _ in range(NC_)]
        UT = Rot(P, "p3_u", 3, [128, 512 + CW - 1], BF16)
        SQ = Rot(P, "p3_sq", 2, [128, 512], BF16)
        C16 = Rot(P, "p3_c16", 2, [128, 512], BF16)
        TT = Rot(P, "p3_tt", 2, [128, 512], F32)
        TMP = Rot(P, "p3_t", 6, [128, 512], F32)
        STG = Rot(P, "p3_z", 3, [128, 512], BF16)
        for c in range(NC_):
            o = c * CW * 128
            col = pv0 + PV_CWT + c * CW
            P.op("dve", lambda e, o=o, col=col: e.tensor_tensor(
                out=dg[:, o:o + CW * 128].rearrange("p (j m) -> p j m", j=CW),
                in0=ident_f.unsqueeze(1).broadcast_to([128, CW, 128]),
                in1=pvec[:, col:col + CW].unsqueeze(2).broadcast_to([128, CW, 128]), op=ALU.mult),
                 reads=[B_cst], writes=[B_dg])
        for t0 in range(t_start, EXT, 512):
            if dbg_range is not None and not (dbg_range[0] <= t0 < dbg_range[1]):
                continue
            ps1, b1 = PSA.next()
            ps2, b2 = PSA.next()
            for c in range(NC_):
                ut, bu = UT.next()
                P.dma("sp", lambda e, ut=ut, c=c, t0=t0: e.dma_start(
                    out=ut[:], in_=uT_d[c * 128:(c + 1) * 128, t0 - (CW - 1):t0 + 512]), bu, writes=[bu])
                pc, bpc = PS.next()
                for j in range(CW):
                    o = (c * CW + j) * 128
                    P.op("pe", lambda e, pc=pc, o=o, ut=ut, j=j: e.matmul(
                        pc[:], lhsT=dg[:, o:o + 128], rhs=ut[:, j:j + 512], start=(j == 0), stop=(j == CW - 1)),
                         reads=[B_dg, bu], writes=[bpc], signal=(j == CW - 1))
                bcol = pv0 + PV_CB + c
                P.op("act", lambda e, c=c, pc=pc, bcol=bcol: e.activation(
                    out=cbuf[:, c, :], in_=pc[:], func=AF.Identity, bias=pvec[:, bcol:bcol + 1]),
                     reads=[bpc, B_cst], writes=[B_c[c]])
                sq, bq = SQ.next()
                P.op("act", lambda e, sq=sq, c=c: e.activation(out=sq[:], in_=cbuf[:, c, :], func=AF.Square),
                     reads=[B_c[c]], writes=[bq])
                c16, b16 = C16.next()
                P.op("act", lambda e, c16=c16, pc=pc, bcol=bcol: e.activation(
                    out=c16[:], in_=pc[:], func=AF.Identity, bias=pvec[:, bcol:bcol + 1]),
                     reads=[bpc, B_cst], writes=[b16])
                P.op("pe", lambda e, ps1=ps1, c16=c16, c=c: e.matmul(ps1[:], lhsT=ones_b, rhs=c16[:],
                                                                    start=(c == 0), stop=(c == NC_ - 1)),
                     reads=[b16, B_cst], writes=[b1])
                P.op("pe", lambda e, ps2=ps2, sq=sq, c=c: e.matmul(ps2[:], lhsT=ones_b, rhs=sq[:],
                                                                   start=(c == 0), stop=(c == NC_ - 1)),
                     reads=[bq, B_cst], writes=[b2])
            mean, bm = TMP.next()
            m2, bm2 = TMP.next()
            var, bvr = TMP.next()
            rs, brs = TMP.next()
            P.op("act", lambda e, mean=mean, ps1=ps1: e.activation(out=mean[:], in_=ps1[:], func=AF.Copy, scale=1.0 / D),
                 reads=[b1], writes=[bm])
            P.op("dve", lambda e, m2=m2, mean=mean: e.tensor_tensor(out=m2[:], in0=mean[:], in1=mean[:], op=ALU.mult),
                 reads=[bm], writes=[bm2])
            P.op("dve", lambda e, var=var, ps2=ps2, m2=m2: e.scalar_tensor_tensor(
                out=var[:], in0=ps2[:], scalar=1.0 / D, in1=m2[:], op0=ALU.mult, op1=ALU.subtract),
                 reads=[b2, bm2], writes=[bvr])
            P.op("act", lambda e, var=var: e.activation(out=var[:], in_=var[:], func=AF.Ln, bias=EPS),
                 reads=[bvr], writes=[bvr])
            P.op("act", lambda e, rs=rs, var=var: e.activation(out=rs[:], in_=var[:], func=AF.Exp, scale=-0.5),
                 reads=[bvr], writes=[brs])
            for c in range(NC_):
                tt, bt = TT.next()
                P.op("dve", lambda e, tt=tt, c=c, mean=mean: e.tensor_tensor(
                    out=tt[:], in0=cbuf[:, c, :], in1=mean[:], op=ALU.subtract), reads=[B_c[c], bm], writes=[bt])
                P.op("dve", lambda e, tt=tt, rs=rs: e.tensor_tensor(out=tt[:], in0=tt[:], in1=rs[:], op=ALU.mult),
                     reads=[bt, brs], writes=[bt])
                zt, bz = STG.next()
                gcol = pv0 + PV_LG + c
                bcol = pv0 + PV_LB + c
                P.op("act", lambda e, zt=zt, tt=tt, gcol=gcol, bcol=bcol: e.activation(
                    out=zt[:], in_=tt[:], func=AF.Silu, scale=pvec[:, gcol:gcol + 1], bias=pvec[:, bcol:bcol + 1]),
                     reads=[bt, B_cst], writes=[bz])
                P.dma("sp", lambda e, zt=zt, c=c, t0=t0: e.dma_start(
                    out=zT_d[c * 128:(c + 1) * 128, t0:t0 + 512], in_=zt[:]), bz, reads=[bz])
        P.phase_end()

    def phase4(l, src_T, t_start):
        TB = 1024
        nsb = TB // 512
        WGH[0] = WGPool(4, 16 * 512)
        oT = P.sb("p4_o", [128, NH, TB], BF16)
        zT = P.sb("p4_z", [128, NC_, TB], BF16)
        mT = P.sb("p4_m", [128, NC_, TB], BF16)
        B_o, B_z, B_m = Buf(), Buf(), Buf()
        GT = Rot(P, "p4_g", 4, [128, 512], F32)
        XI = Rot(P, "p4_x", 3, [128, 512], F32)
        TMP = Rot(P, "p4_t", 4, [128, 512], F32)
        STF = Rot(P, "p4_s", 3, [128, 512], F32)
        for t0 in range(t_start, EXT, TB):
            if dbg_range is not None and not (dbg_range[0] <= t0 < dbg_range[1]):
                continue
            P.dma("sp", lambda e, t0=t0: e.dma_start(
                out=oT[:], in_=oT_d[:, t0:t0 + TB].rearrange("(h p) t -> p h t", p=128)), B_o, writes=[B_o])
            P.dma("sp", lambda e, t0=t0: e.dma_start(
                out=zT[:], in_=zT_d[:, t0:t0 + TB].rearrange("(h p) t -> p h t", p=128)), B_z, writes=[B_z])
            for nbk in range(4):
                wa, ba = load_granule(w_attn_o[l], NH, nbk * 512, 512)
                wc, bc = load_granule(w_conv_o[l], NC_, nbk * 512, 512)
                for m in range(4):
                    ch = nbk * 4 + m
                    for sb in range(nsb):
                        tsl = slice(sb * 512, (sb + 1) * 512)
                        ts = t0 + sb * 512
                        pa, bpa = PS.next()
                        pc, bpc = PS.next()
                        for k in range(NH):
                            P.op("pe", lambda e, pa=pa, wa=wa, k=k, m=m, tsl=tsl: e.matmul(
                                pa[:], lhsT=wa[:, k, m * 128:(m + 1) * 128], rhs=oT[:, k, tsl],
                                start=(k == 0), stop=(k == NH - 1)), reads=[ba, B_o], writes=[bpa], signal=(k == NH - 1))
                        for k in range(NC_):
                            P.op("pe", lambda e, pc=pc, wc=wc, k=k, m=m, tsl=tsl: e.matmul(
                                pc[:], lhsT=wc[:, k, m * 128:(m + 1) * 128], rhs=zT[:, k, tsl],
                                start=(k == 0), stop=(k == NC_ - 1)), reads=[bc, B_z], writes=[bpc], signal=(k == NC_ - 1))
                        ga, bga = GT.next()
                        gc, bgc = GT.next()
                        P.dma("sp", lambda e, ga=ga, ch=ch, ts=ts: e.dma_start(
                            out=ga[:], in_=gt_d[ch * 128:(ch + 1) * 128, ts:ts + 512]), bga, writes=[bga])
                        P.dma("sp", lambda e, gc=gc, ch=ch, ts=ts: e.dma_start(
                            out=gc[:], in_=gt_d[D + ch * 128:D + (ch + 1) * 128, ts:ts + 512]), bgc, writes=[bgc])
                        t1, bt1 = TMP.next()
                        t2, bt2 = TMP.next()
                        P.op("dve", lambda e, t1=t1, pa=pa, ga=ga: e.tensor_tensor(out=t1[:], in0=pa[:], in1=ga[:], op=ALU.mult),
                             reads=[bpa, bga], writes=[bt1])
                        P.op("dve", lambda e, t2=t2, pc=pc, gc=gc: e.tensor_tensor(out=t2[:], in0=pc[:], in1=gc[:], op=ALU.mult),
                             reads=[bpc, bgc], writes=[bt2])
                        P.op("dve", lambda e, t1=t1, t2=t2, ch=ch, tsl=tsl: e.tensor_tensor(
                            out=mT[:, ch, tsl], in0=t1[:], in1=t2[:], op=ALU.add), reads=[bt1, bt2], writes=[B_m])
            for nbk in range(4):
                wo, bo = load_granule(w_out[l], NC_, nbk * 512, 512)
                for m in range(4):
                    ch = nbk * 4 + m
                    for sb in range(nsb):
                        tsl = slice(sb * 512, (sb + 1) * 512)
                        ts = t0 + sb * 512
                        po, bpo = PS.next()
                        for k in range(NC_):
                            P.op("pe", lambda e, po=po, wo=wo, k=k, m=m, tsl=tsl: e.matmul(
                                po[:], lhsT=wo[:, k, m * 128:(m + 1) * 128], rhs=mT[:, k, tsl],
                                start=(k == 0), stop=(k == NC_ - 1)), reads=[bo, B_m], writes=[bpo], signal=(k == NC_ - 1))
                        xi, bxi = XI.next()
                        P.dma("sp", lambda e, xi=xi, ch=ch, ts=ts: e.dma_start(
                            out=xi[:], in_=src_T[ch * 128:(ch + 1) * 128, ts:ts + 512]), bxi, writes=[bxi])
                        st, bs = STF.next()
                        P.op("dve", lambda e, st=st, po=po, xi=xi: e.tensor_tensor(out=st[:], in0=po[:], in1=xi[:], op=ALU.add),
                             reads=[bpo, bxi], writes=[bs])
                        P.dma("sp", lambda e, st=st, ch=ch, ts=ts: e.dma_start(
                            out=x1T_d[ch * 128:(ch + 1) * 128, ts:ts + 512], in_=st[:]), bs, reads=[bs])
        P.phase_end()

    def ffn_norm(l, t0, TB, hT, B_hT, XI, SQ, TMP, rstd, B_rstd, extra=None):
        pv0 = l * PV_L
        for sb in range(TB // 512):
            ts = t0 + sb * 512
            tsl = slice(sb * 512, (sb + 1) * 512)
            pss, bss = PSA.next()
            for c in range(NC_):
                xt, bx = XI.next()
                P.dma("sp", lambda e, xt=xt, c=c, ts=ts: e.dma_start(
                    out=xt[:], in_=x1T_d[c * 128:(c + 1) * 128, ts:ts + 512]), bx, writes=[bx])
                sq, bq = SQ.next()
                P.op("act", lambda e, sq=sq, xt=xt: e.activation(out=sq[:], in_=xt[:], func=AF.Square),
                     reads=[bx], writes=[bq])
                P.op("pe", lambda e, pss=pss, sq=sq, c=c: e.matmul(pss[:], lhsT=ones_b, rhs=sq[:],
                                                                  start=(c == 0), stop=(c == NC_ - 1)),
                     reads=[bq, B_cst], writes=[bss])
            tm, btm = TMP.next()
            P.op("act", lambda e, tm=tm, pss=pss: e.activation(out=tm[:], in_=pss[:], func=AF.Ln, scale=1.0 / D, bias=EPS),
                 reads=[bss], writes=[btm])
            P.op("act", lambda e, tm=tm, tsl=tsl: e.activation(out=rstd[:, tsl], in_=tm[:], func=AF.Exp, scale=-0.5),
                 reads=[btm], writes=[B_rstd])
            for c in range(NC_):
                xt, bx = XI.next()
                P.dma("sp", lambda e, xt=xt, c=c, ts=ts: e.dma_start(
                    out=xt[:], in_=x1T_d[c * 128:(c + 1) * 128, ts:ts + 512]), bx, writes=[bx])
                gcol = pv0 + PV_NFFN + c
                if extra is None:
                    P.op("dve", lambda e, xt=xt, c=c, tsl=tsl, gcol=gcol: e.scalar_tensor_tensor(
                        out=hT[:, c, tsl], in0=xt[:], scalar=pvec[:, gcol:gcol + 1], in1=rstd[:, tsl],
                        op0=ALU.mult, op1=ALU.mult), reads=[bx, B_rstd, B_cst], writes=[B_hT])
                else:
                    extra(xt, bx, c, sb, tsl, gcol)

    def ffn_expert(w1, w3, w2, TB, hT, B_hT, gT, B_gT, TMP, down_epilogue, mid_hook=None):
        nsb = TB // 512
        for fb in range(DFF // 256):
            g1, b1 = load_granule(w1, NC_, fb * 256, 256)
            g3, b3 = load_granule(w3, NC_, fb * 256, 256)
            for m in range(2):
                fch = fb * 2 + m
                for sb in range(nsb):
                    tsl = slice(sb * 512, (sb + 1) * 512)
                    pa, bpa = PS.next()
                    pb, bpb = PS.next()
                    for k in range(NC_):
                        P.op("pe", lambda e, pa=pa, g1=g1, k=k, m=m, tsl=tsl: e.matmul(
                            pa[:], lhsT=g1[:, k, m * 128:(m + 1) * 128], rhs=hT[:, k, tsl],
                            start=(k == 0), stop=(k == NC_ - 1)), reads=[b1, B_hT], writes=[bpa], signal=(k == NC_ - 1))
                    for k in range(NC_):
                        P.op("pe", lambda e, pb=pb, g3=g3, k=k, m=m, tsl=tsl: e.matmul(
                            pb[:], lhsT=g3[:, k, m * 128:(m + 1) * 128], rhs=hT[:, k, tsl],
                            start=(k == 0), stop=(k == NC_ - 1)), reads=[b3, B_hT], writes=[bpb], signal=(k == NC_ - 1))
                    sl, bsl = TMP.next()
                    P.op("act", lambda e, sl=sl, pa=pa: e.activation(out=sl[:], in_=pa[:], func=AF.Silu),
                         reads=[bpa], writes=[bsl])
                    P.op("dve", lambda e, sl=sl, pb=pb, fch=fch, tsl=tsl: e.tensor_tensor(
                        out=gT[:, fch, tsl], in0=sl[:], in1=pb[:], op=ALU.mult), reads=[bsl, bpb], writes=[B_gT])
        if mid_hook is not None:
            mid_hook()
        KQ = DFF // 128 // 4
        for chp in range(NC_ // 2):
            pos = [PS.next() for _ in range(2 * nsb)]
            for qt in range(4):
                wv, bw = load_granule(w2[qt * KQ * 128:(qt + 1) * KQ * 128, :], KQ, chp * 256, 256)
                for m in range(2):
                    for sb in range(nsb):
                        tsl = slice(sb * 512, (sb + 1) * 512)
                        po, bpo = pos[m * nsb + sb]
                        for k in range(KQ):
                            kk = qt * KQ + k
                            P.op("pe", lambda e, po=po, wv=wv, k=k, kk=kk, m=m, tsl=tsl: e.matmul(
                                po[:], lhsT=wv[:, k, m * 128:(m + 1) * 128], rhs=gT[:, kk, tsl],
                                start=(kk == 0), stop=(kk == 4 * KQ - 1)),
                                 reads=[bw, B_gT], writes=[bpo], signal=(k == KQ - 1))
            for m in range(2):
                for sb in range(nsb):
                    tsl = slice(sb * 512, (sb + 1) * 512)
                    po, bpo = pos[m * nsb + sb]
                    down_epilogue(chp * 2 + m, sb, tsl, po, bpo)

    def phase5_dense(l, t_start, dst_T, dst_off):
        TB = 512
        WGH[0] = WGPool(6, 16 * 256)
        hTs = [P.sb("p5_h%d" % i, [128, NC_, TB], BF16) for i in range(2)]
        B_hTs = [Buf(), Buf()]
        gT = P.sb("p5_g", [128, DFF // 128, TB], BF16)
        B_gT = Buf()
        rstds = [P.sb("p5_r%d" % i, [128, TB], F32) for i in range(2)]
        B_rstds = [Buf(), Buf()]
        XI = Rot(P, "p5_x", 3, [128, 512], F32)
        XN = Rot(P, "p5_xn", 3, [128, 512], F32)
        SQ = Rot(P, "p5_q", 2, [128, 512], BF16)
        TMP = Rot(P, "p5_t", 3, [128, 512], F32)
        TMN = Rot(P, "p5_tn", 2, [128, 512], F32)
        STF = Rot(P, "p5_s", 3, [128, 512], F32)
        blocks = [t0 for t0 in range(t_start, EXT, TB)
                  if dbg_range is None or (dbg_range[0] <= t0 < dbg_range[1])]

        def norm(bi):
            ffn_norm(l, blocks[bi], TB, hTs[bi % 2], B_hTs[bi % 2], XN, SQ, TMN, rstds[bi % 2], B_rstds[bi % 2])

        if blocks:
            norm(0)
        for bi, t0 in enumerate(blocks):
            halo_blk = (l == 0 and t0 < OWN0)

            def epi(ch, sb, tsl, po, bpo, t0=t0, halo_blk=halo_blk):
                ts = t0 + sb * 512
                xi, bxi = XI.next()
                P.dma("sp", lambda e, xi=xi, ch=ch, ts=ts: e.dma_start(
                    out=xi[:], in_=x1T_d[ch * 128:(ch + 1) * 128, ts:ts + 512]), bxi, writes=[bxi])
                st, bs = STF.next()
                P.op("dve", lambda e, st=st, po=po, xi=xi: e.tensor_tensor(out=st[:], in0=po[:], in1=xi[:], op=ALU.add),
                     reads=[bpo, bxi], writes=[bs])
                if halo_blk:
                    P.op("dve", lambda e, st=st: e.tensor_scalar(out=st[:], in0=st[:], scalar1=hmask[:, 0:1], scalar2=None,
                                                                 op0=ALU.mult), reads=[bs, B_cst], writes=[bs])
                P.dma("sp", lambda e, st=st, ch=ch, ts=ts: e.dma_start(
                    out=dst_T[ch * 128:(ch + 1) * 128, ts - dst_off:ts - dst_off + 512], in_=st[:]), bs, reads=[bs])

            hook = (lambda bi=bi: norm(bi + 1)) if bi + 1 < len(blocks) else None
            ffn_expert(ffn_w1b, ffn_w3b, ffn_w2b, TB, hTs[bi % 2], B_hTs[bi % 2], gT, B_gT, TMP, epi, mid_hook=hook)
        P.phase_end()

    def phase5_moe(l, t_start, dst_T, dst_off):
        TB = 512
        WGH[0] = WGPool(4, 16 * 256)
        hT = P.sb("p6_h", [128, NC_, TB], BF16)
        gT = P.sb("p6_g", [128, DFF // 128, TB], BF16)
        yacc = P.sb("p6_y", [128, NC_, TB], F32)
        grep = P.sb("p6_gr", [128, NE, TB], F32)
        rt = P.sb("p6_rt", [128, NC_ * NE], F32)
        B_hT, B_gT, B_y, B_gr, B_rt = Buf(), Buf(), Buf(), Buf(), Buf()
        rstd = P.sb("p6_r", [128, TB], F32)
        B_rstd = Buf()
        XI = Rot(P, "p6_x", 3, [128, 512], F32)
        SQ = Rot(P, "p6_q", 2, [128, 512], BF16)
        TMP = Rot(P, "p6_t", 3, [128, 512], F32)
        STF = Rot(P, "p6_s", 3, [128, 512], F32)
        HF = Rot(P, "p6_hf", 2, [128, 512], F32)
        SM = Rot(P, "p6_sm", 8, [128, 16], F32)
        DG = Rot(P, "p6_dg", 2, [128, 128], F32)
        lgT = P.sb("p6_lgT", [128, 512], F32)
        B_lgT = Buf()
        P.dma("sp", lambda e: e.dma_start(out=rt[:], in_=router), B_rt, writes=[B_rt])
        for t0 in range(t_start, EXT, TB):
            if dbg_range is not None and not (dbg_range[0] <= t0 < dbg_range[1]):
                continue
            plT, bplT = PSA.next()

            def extra(xt, bx, c, sb, tsl, gcol):
                hf, bhf = HF.next()
                P.op("dve", lambda e, hf=hf, xt=xt, gcol=gcol, tsl=tsl: e.scalar_tensor_tensor(
                    out=hf[:], in0=xt[:], scalar=pvec[:, gcol:gcol + 1], in1=rstd[:, tsl],
                    op0=ALU.mult, op1=ALU.mult), reads=[bx, B_rstd, B_cst], writes=[bhf])
                P.op("act", lambda e, hf=hf, c=c, tsl=tsl: e.activation(out=hT[:, c, tsl], in_=hf[:], func=AF.Copy),
                     reads=[bhf], writes=[B_hT])
                P.op("pe", lambda e, hf=hf, c=c: e.matmul(
                    plT[0:NE, :], lhsT=rt[:, c * NE:(c + 1) * NE], rhs=hf[:],
                    start=(c == 0), stop=(c == NC_ - 1)), reads=[bhf, B_rt], writes=[bplT])

            ffn_norm(l, t0, TB, hT, B_hT, XI, SQ, TMP, rstd, B_rstd, extra=extra)
            P.op("act", lambda e: e.activation(out=lgT[0:NE, :], in_=plT[0:NE, :], func=AF.Copy),
                 reads=[bplT], writes=[B_lgT])
            for tile in range(4):
                pl_, bl_ = PS.next()
                P.op("pe", lambda e, pl_=pl_, tile=tile: e.matmul(
                    pl_[:, 0:NE], lhsT=lgT[0:NE, tile * 128:(tile + 1) * 128], rhs=ident_f[0:NE, 0:NE],
                    start=True, stop=True), reads=[B_lgT, B_cst], writes=[bl_])
                lg, blg = SM.next()
                m1, bm1 = SM.next()
                m2, bm2 = SM.next()
                e1, be1 = SM.next()
                e2, be2 = SM.next()
                wv, bwv = SM.next()
                gt8, bg8 = SM.next()
                P.op("act", lambda e, lg=lg, pl_=pl_: e.activation(out=lg[:, 0:NE], in_=pl_[:, 0:NE], func=AF.Copy),
                     reads=[bl_], writes=[blg])
                P.op("dve", lambda e, m1=m1, lg=lg: e.reduce_max(out=m1[:, 0:1], in_=lg[:, 0:NE], axis=mybir.AxisListType.X),
                     reads=[blg], writes=[bm1])
                P.op("dve", lambda e, e1=e1, lg=lg, m1=m1: e.tensor_scalar(
                    out=e1[:, 0:NE], in0=lg[:, 0:NE], scalar1=m1[:, 0:1], scalar2=None, op0=ALU.is_equal),
                     reads=[blg, bm1], writes=[be1])
                P.op("dve", lambda e, e2=e2, e1=e1, lg=lg: e.scalar_tensor_tensor(
                    out=e2[:, 0:NE], in0=e1[:, 0:NE], scalar=-1e30, in1=lg[:, 0:NE], op0=ALU.mult, op1=ALU.add),
                     reads=[be1, blg], writes=[be2])
                P.op("dve", lambda e, m2=m2, e2=e2: e.reduce_max(out=m2[:, 0:1], in_=e2[:, 0:NE], axis=mybir.AxisListType.X),
                     reads=[be2], writes=[bm2])
                P.op("dve", lambda e, e2=e2, m2=m2: e.tensor_scalar(
                    out=e2[:, 0:NE], in0=e2[:, 0:NE], scalar1=m2[:, 0:1], scalar2=None, op0=ALU.is_equal),
                     reads=[be2, bm2], writes=[be2])
                P.op("dve", lambda e, wv=wv, m2=m2, m1=m1: e.tensor_tensor(out=wv[:, 0:1], in0=m2[:, 0:1], in1=m1[:, 0:1], op=ALU.subtract),
                     reads=[bm1, bm2], writes=[bwv])
                P.op("act", lambda e, wv=wv: e.activation(out=wv[:, 1:2], in_=wv[:, 0:1], func=AF.Exp), reads=[bwv], writes=[bwv])
                P.op("dve", lambda e, wv=wv: e.tensor_scalar(out=wv[:, 2:3], in0=wv[:, 1:2], scalar1=1.0, scalar2=None, op0=ALU.add),
                     reads=[bwv], writes=[bwv])
                P.op("dve", lambda e, wv=wv: e.reciprocal(out=wv[:, 3:4], in_=wv[:, 2:3]), reads=[bwv], writes=[bwv])
                P.op("dve", lambda e, wv=wv: e.tensor_scalar(out=wv[:, 4:5], in0=wv[:, 3:4], scalar1=-1.0, scalar2=1.0,
                                                             op0=ALU.mult, op1=ALU.add), reads=[bwv], writes=[bwv])
                P.op("dve", lambda e, gt8=gt8, e1=e1, wv=wv: e.tensor_scalar(
                    out=gt8[:, 0:NE], in0=e1[:, 0:NE], scalar1=wv[:, 3:4], scalar2=None, op0=ALU.mult),
                     reads=[be1, bwv], writes=[bg8])
                P.op("dve", lambda e, gt8=gt8, e2=e2, wv=wv: e.scalar_tensor_tensor(
                    out=gt8[:, 0:NE], in0=e2[:, 0:NE], scalar=wv[:, 4:5], in1=gt8[:, 0:NE], op0=ALU.mult, op1=ALU.add),
                     reads=[be2, bwv, bg8], writes=[bg8])
                for ex in range(NE):
                    dgm, bdg = DG.next()
                    P.op("dve", lambda e, dgm=dgm, gt8=gt8, ex=ex: e.tensor_scalar(
                        out=dgm[:], in0=ident_f, scalar1=gt8[:, ex:ex + 1], scalar2=None, op0=ALU.mult),
                         reads=[bg8, B_cst], writes=[bdg])
                    pr, bpr = PS.next()
                    P.op("pe", lambda e, pr=pr, dgm=dgm: e.matmul(pr[:, 0:128], lhsT=ones_f, rhs=dgm[:], start=True, stop=True),
                         reads=[bdg, B_cst], writes=[bpr])
                    P.op("act", lambda e, pr=pr, ex=ex, tile=tile: e.activation(
                        out=grep[:, ex, tile * 128:(tile + 1) * 128], in_=pr[:, 0:128], func=AF.Copy),
                         reads=[bpr], writes=[B_gr])
            for ex in range(NE):
                def epi(ch, sb, tsl, po, bpo, ex=ex):
                    if ex == 0:
                        P.op("dve", lambda e, po=po, ch=ch, ex=ex: e.tensor_tensor(
                            out=yacc[:, ch, :], in0=po[:], in1=grep[:, ex, :], op=ALU.mult),
                             reads=[bpo, B_gr], writes=[B_y])
                    else:
                        tt, bt = TMP.next()
                        P.op("dve", lambda e, tt=tt, po=po, ex=ex: e.tensor_tensor(
                            out=tt[:], in0=po[:], in1=grep[:, ex, :], op=ALU.mult), reads=[bpo, B_gr], writes=[bt])
                        P.op("dve", lambda e, tt=tt, ch=ch: e.tensor_tensor(
                            out=yacc[:, ch, :], in0=yacc[:, ch, :], in1=tt[:], op=ALU.add), reads=[bt, B_y], writes=[B_y])
                ffn_expert(moe_w1[0, ex], moe_w3[0, ex], moe_w2[0, ex], TB, hT, B_hT, gT, B_gT, TMP, epi)
            if debug:
                P.dma("sp", lambda e: e.dma_start(out=dbg_g, in_=grep[:].rearrange("p e t -> p (e t)")), B_gr, reads=[B_gr])
                P.dma("sp", lambda e: e.dma_start(out=dbg_y, in_=yacc[:].rearrange("p c t -> p (c t)")), B_y, reads=[B_y])
            for ch in range(NC_):
                xi, bxi = XI.next()
                P.dma("sp", lambda e, xi=xi, ch=ch, t0=t0: e.dma_start(
                    out=xi[:], in_=x1T_d[ch * 128:(ch + 1) * 128, t0:t0 + 512]), bxi, writes=[bxi])
                st, bs = STF.next()
                P.op("dve", lambda e, st=st, xi=xi, ch=ch: e.tensor_tensor(out=st[:], in0=yacc[:, ch, :], in1=xi[:], op=ALU.add),
                     reads=[B_y, bxi], writes=[bs])
                P.dma("sp", lambda e, st=st, ch=ch, t0=t0: e.dma_start(
                    out=dst_T[ch * 128:(ch + 1) * 128, t0 - dst_off:t0 - dst_off + 512], in_=st[:]), bs, reads=[bs])
        P.phase_end()

    def phase5_moe_sparse(l, t_start, dst_T, dst_off):
        TB = 512
        C = MOE_C
        SL = ((0, 128), (128, C - 128))
        pv0 = l * PV_L
        WGH[0] = WGPool(4, 16 * 256)
        h_tok = P.sb("p7_ht", [128, 4, D], BF16)
        hTe = P.sb("p7_he", [128, NC_, C], BF16)
        gTe = P.sb("p7_ge", [128, DFF // 128, C], BF16)
        oute = P.sb("p7_oe", [128, 2, D], BF16)
        outT = P.sb("p7_oT", [128, NC_, C], BF16)
        B_oT = Buf()
        yacc = P.sb("p7_y", [128, NC_, TB], F32)
        rt = P.sb("p7_rt", [128, NC_ * NE], F32)
        lgT = P.sb("p7_lgT", [128, 512], F32)
        G_all = P.sb("p7_G", [128, 4, NE], F32)
        M_all = P.sb("p7_M", [128, 4, NE], F32)
        Mb_all = P.sb("p7_Mb", [128, 4, NE], BF16)
        R_all = P.sb("p7_R", [128, 4, NE], F32)
        rstd = P.sb("p7_r", [128, TB], F32)
        B_ht, B_he, B_ge, B_oe, B_y, B_rt, B_lgT, B_G, B_R, B_rstd = (Buf() for _ in range(10))
        SEL = Rot(P, "p7_sel", 2, [128, 4, C], BF16)
        SELG = Rot(P, "p7_selg", 2, [128, 4, C], BF16)
        SGT = Rot(P, "p7_sgt", 2, [128, 2, TB], BF16)
        XI = Rot(P, "p7_x", 3, [128, 512], F32)
        SQ = Rot(P, "p7_q", 2, [128, 512], BF16)
        TMP = Rot(P, "p7_t", 3, [128, 512], F32)
        STF = Rot(P, "p7_s", 3, [128, 512], F32)
        HF = Rot(P, "p7_hf", 2, [128, 512], F32)
        HB = Rot(P, "p7_hb", 2, [128, 512], BF16)
        SM = Rot(P, "p7_sm", 8, [128, 16], F32)
        P.dma("sp", lambda e: e.dma_start(out=rt[:], in_=router), B_rt, writes=[B_rt])
        for t0 in range(t_start, EXT, TB):
            if dbg_range is not None and not (dbg_range[0] <= t0 < dbg_range[1]):
                continue
            plT, bplT = PSA.next()

            def extra(xt, bx, c, sb, tsl, gcol):
                hf, bhf = HF.next()
                P.op("dve", lambda e, hf=hf, xt=xt, gcol=gcol, tsl=tsl: e.scalar_tensor_tensor(
                    out=hf[:], in0=xt[:], scalar=pvec[:, gcol:gcol + 1], in1=rstd[:, tsl],
                    op0=ALU.mult, op1=ALU.mult), reads=[bx, B_rstd, B_cst], writes=[bhf])
                P.op("pe", lambda e, hf=hf, c=c: e.matmul(
                    plT[0:NE, :], lhsT=rt[:, c * NE:(c + 1) * NE], rhs=hf[:],
                    start=(c == 0), stop=(c == NC_ - 1)), reads=[bhf, B_rt], writes=[bplT])
                hb, bhb = HB.next()
                P.op("act", lambda e, hb=hb, hf=hf: e.activation(out=hb[:], in_=hf[:], func=AF.Copy),
                     reads=[bhf], writes=[bhb])
                ptr, bptr = PS.next()
                for tile in range(4):
                    P.op("pe", lambda e, ptr=ptr, hb=hb, tile=tile: e.matmul(
                        ptr[:, tile * 128:(tile + 1) * 128], lhsT=hb[:, tile * 128:(tile + 1) * 128], rhs=ident_b,
                        start=True, stop=True), reads=[bhb, B_cst], writes=[bptr], signal=(tile == 3))
                P.op("act", lambda e, ptr=ptr, c=c: e.activation(
                    out=h_tok[:, :, c * 128:(c + 1) * 128], in_=ptr[:].rearrange("p (i f) -> p i f", i=4), func=AF.Copy),
                     reads=[bptr], writes=[B_ht])

            ffn_norm(l, t0, TB, None, None, XI, SQ, TMP, rstd, B_rstd, extra=extra)
            P.op("act", lambda e: e.activation(out=lgT[0:NE, :], in_=plT[0:NE, :], func=AF.Copy),
                 reads=[bplT], writes=[B_lgT])
            for tile in range(4):
                pl_, bl_ = PS.next()
                P.op("pe", lambda e, pl_=pl_, tile=tile: e.matmul(
                    pl_[:, 0:NE], lhsT=lgT[0:NE, tile * 128:(tile + 1) * 128], rhs=ident_f[0:NE, 0:NE],
                    start=True, stop=True), reads=[B_lgT, B_cst], writes=[bl_])
                lg, blg = SM.next()
                m1, bm1 = SM.next()
                m2, bm2 = SM.next()
                e1, be1 = SM.next()
                e2, be2 = SM.next()
                wv, bwv = SM.next()
                P.op("act", lambda e, lg=lg, pl_=pl_: e.activation(out=lg[:, 0:NE], in_=pl_[:, 0:NE], func=AF.Copy),
                     reads=[bl_], writes=[blg])
                P.op("dve", lambda e, m1=m1, lg=lg: e.reduce_max(out=m1[:, 0:1], in_=lg[:, 0:NE], axis=mybir.AxisListType.X),
                     reads=[blg], writes=[bm1])
                P.op("dve", lambda e, e1=e1, lg=lg, m1=m1: e.tensor_scalar(
                    out=e1[:, 0:NE], in0=lg[:, 0:NE], scalar1=m1[:, 0:1], scalar2=None, op0=ALU.is_equal),
                     reads=[blg, bm1], writes=[be1])
                P.op("dve", lambda e, e2=e2, e1=e1, lg=lg: e.scalar_tensor_tensor(
                    out=e2[:, 0:NE], in0=e1[:, 0:NE], scalar=-1e30, in1=lg[:, 0:NE], op0=ALU.mult, op1=ALU.add),
                     reads=[be1, blg], writes=[be2])
                P.op("dve", lambda e, m2=m2, e2=e2: e.reduce_max(out=m2[:, 0:1], in_=e2[:, 0:NE], axis=mybir.AxisListType.X),
                     reads=[be2], writes=[bm2])
                P.op("dve", lambda e, e2=e2, m2=m2: e.tensor_scalar(
                    out=e2[:, 0:NE], in0=e2[:, 0:NE], scalar1=m2[:, 0:1], scalar2=None, op0=ALU.is_equal),
                     reads=[be2, bm2], writes=[be2])
                P.op("dve", lambda e, wv=wv, m2=m2, m1=m1: e.tensor_tensor(out=wv[:, 0:1], in0=m2[:, 0:1], in1=m1[:, 0:1], op=ALU.subtract),
                     reads=[bm1, bm2], writes=[bwv])
                P.op("act", lambda e, wv=wv: e.activation(out=wv[:, 1:2], in_=wv[:, 0:1], func=AF.Exp), reads=[bwv], writes=[bwv])
                P.op("dve", lambda e, wv=wv: e.tensor_scalar(out=wv[:, 2:3], in0=wv[:, 1:2], scalar1=1.0, scalar2=None, op0=ALU.add),
                     reads=[bwv], writes=[bwv])
                P.op("dve", lambda e, wv=wv: e.reciprocal(out=wv[:, 3:4], in_=wv[:, 2:3]), reads=[bwv], writes=[bwv])
                P.op("dve", lambda e, wv=wv: e.tensor_scalar(out=wv[:, 4:5], in0=wv[:, 3:4], scalar1=-1.0, scalar2=1.0,
                                                             op0=ALU.mult, op1=ALU.add), reads=[bwv], writes=[bwv])
                P.op("dve", lambda e, e1=e1, wv=wv, tile=tile: e.tensor_scalar(
                    out=G_all[:, tile, :], in0=e1[:, 0:NE], scalar1=wv[:, 3:4], scalar2=None, op0=ALU.mult),
                     reads=[be1, bwv], writes=[B_G])
                P.op("dve", lambda e, e2=e2, wv=wv, tile=tile: e.scalar_tensor_tensor(
                    out=G_all[:, tile, :], in0=e2[:, 0:NE], scalar=wv[:, 4:5], in1=G_all[:, tile, :], op0=ALU.mult, op1=ALU.add),
                     reads=[be2, bwv, B_G], writes=[B_G])
                P.op("dve", lambda e, e1=e1, e2=e2, tile=tile: e.tensor_tensor(
                    out=M_all[:, tile, :], in0=e1[:, 0:NE], in1=e2[:, 0:NE], op=ALU.add), reads=[be1, be2], writes=[B_G])
                P.op("dve", lambda e, tile=tile: e.tensor_copy(out=Mb_all[:, tile, :], in_=M_all[:, tile, :]),
                     reads=[B_G], writes=[B_G])
            for tile in range(4):
                pr, bpr = PS.next()
                for j in range(tile + 1):
                    P.op("pe", lambda e, pr=pr, j=j, tile=tile: e.matmul(
                        pr[:, 0:NE], lhsT=(tri_b if j == tile else ones_b), rhs=Mb_all[:, j, :],
                        start=(j == 0), stop=(j == tile)), reads=[B_G, B_cst], writes=[bpr], signal=(j == tile))
                P.op("act", lambda e, pr=pr, tile=tile: e.activation(out=R_all[:, tile, :], in_=pr[:, 0:NE], func=AF.Copy),
                     reads=[bpr], writes=[B_R])
            for ex in range(NE):
                sel, bsel = SEL.next()
                selg, bselg = SELG.next()
                for tile in range(4):
                    P.op("dve", lambda e, sel=sel, tile=tile, ex=ex: e.tensor_scalar(
                        out=sel[:, tile, :], in0=iota_f[:, 0:C], scalar1=R_all[:, tile, ex:ex + 1],
                        scalar2=M_all[:, tile, ex:ex + 1], op0=ALU.is_equal, op1=ALU.mult),
                         reads=[B_R, B_G, B_cst], writes=[bsel])
                    P.op("dve", lambda e, selg=selg, tile=tile, ex=ex: e.tensor_scalar(
                        out=selg[:, tile, :], in0=iota_f[:, 0:C], scalar1=R_all[:, tile, ex:ex + 1],
                        scalar2=G_all[:, tile, ex:ex + 1], op0=ALU.is_equal, op1=ALU.mult),
                         reads=[B_R, B_G, B_cst], writes=[bselg])
                for c in range(NC_):
                    pg, bpg = PS.next()
                    for tile in range(4):
                        P.op("pe", lambda e, pg=pg, c=c, tile=tile, sel=sel: e.matmul(
                            pg[:, 0:C], lhsT=h_tok[:, tile, c * 128:(c + 1) * 128], rhs=sel[:, tile, :],
                            start=(tile == 0), stop=(tile == 3)), reads=[B_ht, bsel], writes=[bpg], signal=(tile == 3))
                    P.op("act", lambda e, pg=pg, c=c: e.activation(out=hTe[:, c, :], in_=pg[:, 0:C], func=AF.Copy),
                         reads=[bpg], writes=[B_he])
                w1, w3, w2 = moe_w1b[ex], moe_w3b[ex], moe_w2b[ex]
                for fb in range(DFF // 256):
                    g1, b1 = load_granule(w1, NC_, fb * 256, 256)
                    g3, b3 = load_granule(w3, NC_, fb * 256, 256)
                    for m in range(2):
                        fch = fb * 2 + m
                        pa, bpa = PS.next()
                        pb, bpb = PS.next()
                        for k in range(NC_):
                            P.op("pe", lambda e, pa=pa, g1=g1, k=k, m=m: e.matmul(
                                pa[:, 0:C], lhsT=g1[:, k, m * 128:(m + 1) * 128], rhs=hTe[:, k, :],
                                start=(k == 0), stop=(k == NC_ - 1)), reads=[b1, B_he], writes=[bpa], signal=(k == NC_ - 1))
                        for k in range(NC_):
                            P.op("pe", lambda e, pb=pb, g3=g3, k=k, m=m: e.matmul(
                                pb[:, 0:C], lhsT=g3[:, k, m * 128:(m + 1) * 128], rhs=hTe[:, k, :],
                                start=(k == 0), stop=(k == NC_ - 1)), reads=[b3, B_he], writes=[bpb], signal=(k == NC_ - 1))
                        sl, bsl = TMP.next()
                        P.op("act", lambda e, sl=sl, pa=pa: e.activation(out=sl[:, 0:C], in_=pa[:, 0:C], func=AF.Silu),
                             reads=[bpa], writes=[bsl])
                        P.op("dve", lambda e, sl=sl, pb=pb, fch=fch: e.tensor_tensor(
                            out=gTe[:, fch, :], in0=sl[:, 0:C], in1=pb[:, 0:C], op=ALU.mult), reads=[bsl, bpb], writes=[B_ge])
                KQ = DFF // 128 // 4
                for chp in range(NC_ // 2):
                    pos = [PS.next() for _ in range(2)]
                    for qt in range(4):
                        wv_, bw = load_granule(w2[qt * KQ * 128:(qt + 1) * KQ * 128, :], KQ, chp * 256, 256)
                        for m in range(2):
                            po, bpo = pos[m]
                            for k in range(KQ):
                                kk = qt * KQ + k
                                P.op("pe", lambda e, po=po, wv_=wv_, k=k, kk=kk, m=m: e.matmul(
                                    po[:, 0:C], lhsT=wv_[:, k, m * 128:(m + 1) * 128], rhs=gTe[:, kk, :],
                                    start=(kk == 0), stop=(kk == 4 * KQ - 1)),
                                     reads=[bw, B_ge], writes=[bpo], signal=(k == KQ - 1))
                    for m in range(2):
                        po, bpo = pos[m]
                        P.op("act", lambda e, po=po, chp=chp, m=m: e.activation(
                            out=outT[:, chp * 2 + m, :], in_=po[:, 0:C], func=AF.Copy), reads=[bpo], writes=[B_oT])
                for j, (s0, sn) in enumerate(SL):
                    for cg in range(4):
                        pt2, bpt2 = PS.next()
                        for i in range(4):
                            ch = cg * 4 + i
                            P.op("pe", lambda e, pt2=pt2, i=i, ch=ch, s0=s0, sn=sn: e.matmul(
                                pt2[0:sn, i * 128:(i + 1) * 128], lhsT=outT[:, ch, s0:s0 + sn], rhs=ident_b,
                                start=True, stop=True), reads=[B_oT, B_cst], writes=[bpt2], signal=(i == 3))
                        P.op("act", lambda e, pt2=pt2, j=j, sn=sn, cg=cg: e.activation(
                            out=oute[0:sn, j, cg * 512:(cg + 1) * 512], in_=pt2[0:sn, :], func=AF.Copy),
                             reads=[bpt2], writes=[B_oe])
                sgt, bsgt = SGT.next()
                for j, (s0, sn) in enumerate(SL):
                    pt_, bpt_ = PS.next()
                    for tile in range(4):
                        P.op("pe", lambda e, pt_=pt_, selg=selg, tile=tile, s0=s0, sn=sn: e.matmul(
                            pt_[0:sn, tile * 128:(tile + 1) * 128], lhsT=selg[:, tile, s0:s0 + sn], rhs=ident_b,
                            start=True, stop=True), reads=[bselg, B_cst], writes=[bpt_], signal=(tile == 3))
                    P.op("act", lambda e, pt_=pt_, sgt=sgt, j=j, sn=sn: e.activation(
                        out=sgt[0:sn, j, :], in_=pt_[0:sn, :], func=AF.Copy), reads=[bpt_], writes=[bsgt])
                for ch in range(NC_):
                    pc_, bpc_ = PS.next()
                    for j, (s0, sn) in enumerate(SL):
                        P.op("pe", lambda e, pc_=pc_, j=j, sn=sn, ch=ch, sgt=sgt: e.matmul(
                            pc_[:], lhsT=oute[0:sn, j, ch * 128:(ch + 1) * 128], rhs=sgt[0:sn, j, :],
                            start=(j == 0), stop=(j == 1)), reads=[B_oe, bsgt], writes=[bpc_], signal=(j == 1))
                    if ex == 0:
                        P.op("act", lambda e, pc_=pc_, ch=ch: e.activation(out=yacc[:, ch, :], in_=pc_[:], func=AF.Copy),
                             reads=[bpc_], writes=[B_y])
                    else:
                        P.op("dve", lambda e, pc_=pc_, ch=ch: e.tensor_tensor(
                            out=yacc[:, ch, :], in0=yacc[:, ch, :], in1=pc_[:], op=ALU.add), reads=[bpc_, B_y], writes=[B_y])
            for ch in range(NC_):
                xi, bxi = XI.next()
                P.dma("sp", lambda e, xi=xi, ch=ch, t0=t0: e.dma_start(
                    out=xi[:], in_=x1T_d[ch * 128:(ch + 1) * 128, t0:t0 + 512]), bxi, writes=[bxi])
                st, bs = STF.next()
                P.op("dve", lambda e, st=st, xi=xi, ch=ch: e.tensor_tensor(out=st[:], in0=yacc[:, ch, :], in1=xi[:], op=ALU.add),
                     reads=[B_y, bxi], writes=[bs])
                P.dma("sp", lambda e, st=st, ch=ch, t0=t0: e.dma_start(
                    out=dst_T[ch * 128:(ch + 1) * 128, t0 - dst_off:t0 - dst_off + 512], in_=st[:]), bs, reads=[bs])
        P.phase_end()

    src = xT_in
    for l in layers:
        t_part0 = 0 if l == 0 else HALO0
        t_full0 = HALO0 if l == 0 else OWN0
        if not (dbg_blocks == []):
            phase1(l, src, t_part0, t_full0)
        if stop_after == ("p1", l):
            break
        phase2(l, t_full0)
        if stop_after == ("p2", l):
            break
        phase3(l, t_full0)
        if stop_after == ("p3", l):
            break
        precast_issue(n_pre_ffn if (l == 0 and not dbg_moe_l0) else len(pre_list))
        phase4(l, src, t_full0)
        if stop_after == ("p4", l):
            break
        if l == 0:
            phase5_dense(l, t_full0, x2T_d, 0)
            if dbg_moe_l0:
                (phase5_moe_sparse if sparse else phase5_moe)(l, t_full0, yT_out, OWN0)
        else:
            (phase5_moe_sparse if sparse else phase5_moe)(l, t_full0, yT_out, OWN0)
        src = x2T_d

    P.barrier()
    P.emit()
    es.close()
    return nc, P


def _chunked(v):
    return np.ascontiguousarray(np.asarray(v, np.float32).reshape(-1, 128).T)


def host_consts():
    ones = np.ones((128, 128), np.float32)
    ident = np.eye(128, dtype=np.float32)
    rmat = np.zeros((128, 128), np.float32)
    for e in range(16):
        rmat[e + 16, e] = -1.0
        rmat[e, e + 16] = 1.0
    k = np.arange(128)[:, None]
    q = np.arange(128)[None, :]
    m_prev = (k >= q).astype(np.float32)
    m_own = (k <= q).astype(np.float32)
    tri = (k < q).astype(np.float32)
    iota = np.broadcast_to(np.arange(256, dtype=np.float32)[None, :], (128, 256))
    return np.ascontiguousarray(np.concatenate([ones, ident, rmat, m_prev, m_own, tri, iota], axis=1))


def host_pvec(inp):
    cols = []
    for l in range(2):
        blk = [_chunked(inp["norm_mix"][l]), _chunked(inp["norm_ffn"][l]), _chunked(inp["conv_b"][l]),
               _chunked(inp["conv_ln_g"][l]), _chunked(inp["conv_ln_b"][l]), _chunked(inp["gate_b"][l]),
               np.asarray(inp["q_norm"][l], np.float32).reshape(128, 1),
               np.asarray(inp["k_norm"][l], np.float32).reshape(128, 1)]
        cw = np.asarray(inp["conv_w"][l], np.float32).reshape(CW, D)
        cwt = cw.T.reshape(NC_, 128, CW).transpose(1, 0, 2).reshape(128, NC_ * CW)
        blk.append(cwt)
        cols.append(np.concatenate(blk, axis=1))
    pv = np.concatenate(cols, axis=1)
    assert pv.shape == (128, 2 * PV_L), pv.shape
    return np.ascontiguousarray(pv.astype(np.float32))


def host_core_inputs(inp, core):
    b, half = core // 2, core % 2
    s0 = half * 4096
    x = np.asarray(inp["x"], np.float32)
    t = np.arange(EXT) + (s0 - 4096)
    valid = t >= 0
    xT = np.zeros((D, EXT), np.float32)
    xT[:, valid] = x[b, t[valid], :].T
    hm = np.full((128, 1), 1.0 if half == 1 else 0.0, np.float32)
    inv = np.power(np.float32(500000.0), -np.arange(16, dtype=np.float32) * np.float32(2.0) / np.float32(32.0)).astype(np.float32)
    pos = t.astype(np.float32)
    ang = (pos[None, :] * inv[:, None]).astype(np.float32)
    cosT = np.ones((128, EXT), np.float32)
    sinT = np.zeros((128, EXT), np.float32)
    cosT[0:16] = np.cos(ang)
    cosT[16:32] = np.cos(ang)
    sinT[0:16] = np.sin(ang)
    sinT[16:32] = np.sin(ang)
    return {"xT": xT, "hmask": hm, "cosT": cosT, "sinT": sinT}


def host_shared_inputs(inp, use_ffn=True, use_moe=True):
    sh = {"cst": host_consts(), "pvec": host_pvec(inp)}
    for k in ("w_in", "w_attn_o", "w_conv_o", "w_out"):
        sh[k] = np.ascontiguousarray(np.asarray(inp[k], np.float32))
    if use_ffn:
        for k in ("ffn_w1", "ffn_w3", "ffn_w2"):
            sh[k] = np.ascontiguousarray(np.asarray(inp[k], np.float32))
    if use_moe:
        r = np.asarray(inp["router"], np.float32)[0]
        sh["router"] = np.ascontiguousarray(r.reshape(NC_, 128, NE).transpose(1, 0, 2).reshape(128, NC_ * NE))
        for k in ("moe_w1", "moe_w3", "moe_w2"):
            sh[k] = np.ascontiguousarray(np.asarray(inp[k], np.float32))
    return sh


def kernel(**inputs):
    nc, _ = build_program()
    sh = host_shared_inputs(inputs)
    in_maps = []
    for core in range(8):
        m = dict(sh)
        m.update(host_core_inputs(inputs, core))
        in_maps.append(m)
    res = run_bass_kernel_spmd(nc, in_maps, core_ids=list(range(8)))
    out = np.empty((BATCH, SEQ, D), np.float32)
    for core in range(8):
        b, half = core // 2, core % 2
        out[b, half * 4096:(half + 1) * 4096, :] = np.asarray(res.results[core]["yT"]).T
    return out
```
